# Optimizing a Trainium2 kernel written in Bass

```python
import jax
import jax.numpy as jnp
from jax import lax
import numpy as np

D_MODEL = 2048
BATCH = 4
SEQ = 2048
DEPTH = 2
DEC_BATCH = 8
DEC_SEQ = 1
PAST_LEN = 16384
PAGE_SIZE = 128

N_A_LAYERS = DEPTH // 2
N_B_LAYERS = DEPTH - N_A_LAYERS
N_DENSE = (DEPTH + 1) // 2
N_MOE = DEPTH // 2

MLSTM_HEADS = 4
MLSTM_DK = D_MODEL // (2 * MLSTM_HEADS)
MLSTM_DV = D_MODEL // MLSTM_HEADS
MLSTM_CHUNK = 64
GATE_SOFTCAP = 15.0
MLSTM_PROJ = 2 * MLSTM_HEADS * MLSTM_DK + 2 * MLSTM_HEADS * MLSTM_DV + 2 * MLSTM_HEADS

ATTN_HEAD_DIM = 128
ATTN_HEADS = D_MODEL // ATTN_HEAD_DIM
DILATED_GROUPS = ((128, 1), (512, 4), (2048, 16))
N_GROUPS = len(DILATED_GROUPS)
ATTN_WIDTH = ATTN_HEADS * ATTN_HEAD_DIM

D_FF = 5632
N_EXPERTS = 8
TOP_K = 2
D_FF_EXPERT = 2816
EPS = 1e-6

kernel_name = 'yoco_mlstm_dilated_swa_moe_step'

F32 = jnp.float32


def rmsnorm(x, g):
    xf = x.astype(F32)
    y = xf * lax.rsqrt(jnp.mean(xf * xf, axis=-1, keepdims=True) + EPS)
    return (y * g.astype(F32)).astype(x.dtype)


def softcap(x):
    return GATE_SOFTCAP * jnp.tanh(x / GATE_SOFTCAP)


def swiglu(x, w_gate, w_up, w_down):
    return (jax.nn.silu(x @ w_gate) * (x @ w_up)) @ w_down


def moe_swiglu(x, w_router, b_router, w_gate, w_up, w_down):
    logits = (x @ w_router).astype(F32) + b_router.astype(F32)
    probs = jax.nn.softmax(logits, axis=-1)
    top_p, top_i = lax.top_k(probs, TOP_K)
    top_p = top_p / jnp.sum(top_p, axis=-1, keepdims=True)
    gates = jnp.sum(jax.nn.one_hot(top_i, N_EXPERTS, dtype=F32) * top_p[..., None], axis=-2)
    y = jnp.zeros(x.shape, F32)
    for e in range(N_EXPERTS):
        y = y + gates[..., e:e + 1] * swiglu(x, w_gate[e], w_up[e], w_down[e]).astype(F32)
    return y.astype(x.dtype)


def mlstm_chunk_step(carry, inp):
    C, n, m = carry
    q, k, v, ig, lf = inp
    L = q.shape[2]
    b = jnp.cumsum(lf, axis=-1)
    causal = jnp.tril(jnp.ones((L, L), bool))
    dmat = jnp.where(causal, b[..., :, None] - b[..., None, :] + ig[..., None, :], -jnp.inf)
    inter = b + m[..., None]
    m_t = jnp.maximum(inter, jnp.max(dmat, axis=-1))
    w_intra = jnp.exp(dmat - m_t[..., None])
    w_inter = jnp.exp(inter - m_t)
    s = jnp.einsum('bhtd,bhsd->bhts', q, k) * w_intra
    num = w_inter[..., None] * jnp.einsum('bhtd,bhde->bhte', q, C) + jnp.einsum('bhts,bhse->bhte', s, v)
    den = w_inter * jnp.einsum('bhtd,bhd->bht', q, n) + jnp.sum(s, axis=-1)
    h = num / jnp.maximum(jnp.abs(den), jnp.exp(-m_t))[..., None]
    b_last = b[..., -1]
    g = b_last[..., None] - b + ig
    m_new = jnp.maximum(b_last + m, jnp.max(g, axis=-1))
    decay = jnp.exp(b_last + m - m_new)
    w_k = jnp.exp(g - m_new[..., None])
    kw = k * w_k[..., None]
    C_new = decay[..., None, None] * C + jnp.einsum('bhsd,bhse->bhde', kw, v)
    n_new = decay[..., None] * n + jnp.sum(kw, axis=2)
    return (C_new, n_new, m_new), h


def mlstm_mixer(xn, w_in, b_gates, out_norm, w_out, C0, n0, m0):
    Bn, S, _ = xn.shape
    hk = MLSTM_HEADS * MLSTM_DK
    hv = MLSTM_HEADS * MLSTM_DV
    q, k, v, o, gates = jnp.split(xn @ w_in, [hk, 2 * hk, 2 * hk + hv, 2 * hk + 2 * hv], axis=-1)

    def heads(t, d):
        return t.reshape(Bn, S, MLSTM_HEADS, d).transpose(0, 2, 1, 3).astype(F32)

    q = heads(q, MLSTM_DK)
    k = heads(k, MLSTM_DK) * (MLSTM_DK ** -0.5)
    v = heads(v, MLSTM_DV)
    gates = softcap(gates.astype(F32) + b_gates.astype(F32)).transpose(0, 2, 1)
    ig = gates[:, :MLSTM_HEADS]
    lf = jax.nn.log_sigmoid(gates[:, MLSTM_HEADS:])
    L = MLSTM_CHUNK if S % MLSTM_CHUNK == 0 else S
    nc = S // L

    def chunks(t):
        return jnp.moveaxis(t.reshape(t.shape[:2] + (nc, L) + t.shape[3:]), 2, 0)

    (C, n, m), h = lax.scan(mlstm_chunk_step, (C0.astype(F32), n0.astype(F32), m0.astype(F32)),
                            (chunks(q), chunks(k), chunks(v), chunks(ig), chunks(lf)))
    h = jnp.moveaxis(h, 0, 2).reshape(Bn, MLSTM_HEADS, S, MLSTM_DV).transpose(0, 2, 1, 3)
    h = h * lax.rsqrt(jnp.mean(h * h, axis=-1, keepdims=True) + EPS)
    h = h.reshape(Bn, S, hv) * out_norm.astype(F32) * jax.nn.sigmoid(o.astype(F32))
    return h.astype(xn.dtype) @ w_out, C, n, m


def masked_softmax(s, valid):
    s = jnp.where(valid, s, -jnp.inf)
    mx = jnp.max(s, axis=-1, keepdims=True)
    p = jnp.exp(s - mx)
    l = jnp.sum(p, axis=-1, keepdims=True)
    return p / l, (mx + jnp.log(l))[..., 0]


def band_attention(q, k, v, steps):
    N, T, H, D = q.shape
    bq = steps
    nb = -(-T // bq)
    tail = nb * bq - T
    qb = jnp.pad(q, ((0, 0), (0, tail), (0, 0), (0, 0))).reshape(N, nb, bq, H, D)

    def blocks(t):
        tp = jnp.pad(t, ((0, 0), (bq, tail), (0, 0), (0, 0)))
        prev = tp[:, :nb * bq].reshape(N, nb, bq, H, D)
        cur = tp[:, bq:].reshape(N, nb, bq, H, D)
        return jnp.concatenate([prev, cur], axis=2)

    kb, vb = blocks(k), blocks(v)
    s = jnp.einsum('nbqhd,nbkhd->nbhqk', qb, kb, preferred_element_type=F32) * (D ** -0.5)
    i = jnp.arange(bq)[:, None]
    j = jnp.arange(2 * bq)[None, :]
    dist = bq + i - j
    kpos = (jnp.arange(nb) * bq)[:, None, None] - bq + j[None]
    valid = (dist >= 0) & (dist <= steps) & (kpos >= 0)
    p, lse = masked_softmax(s, valid[None, :, None])
    o = jnp.einsum('nbhqk,nbkhd->nbqhd', p.astype(vb.dtype), vb).reshape(N, nb * bq, H, D)[:, :T]
    lse = lse.transpose(0, 1, 3, 2).reshape(N, nb * bq, H)[:, :T]
    return o, lse


def dilated_prompt(q, k, v, dilation, steps):
    B, S, H, D = q.shape
    sub = S // dilation

    def fold(t):
        return t.reshape(B, sub, dilation, H, D).transpose(0, 2, 1, 3, 4).reshape(B * dilation, sub, H, D)

    o, lse = band_attention(fold(q), fold(k), fold(v), steps)
    o = o.reshape(B, dilation, sub, H, D).transpose(0, 2, 1, 3, 4).reshape(B, S, H, D)
    lse = lse.reshape(B, dilation, sub, H).transpose(0, 2, 1, 3).reshape(B, S, H)
    return o, lse


def dilated_decode(q, k_new, v_new, kv_buf, dilation, steps):
    Lb = kv_buf.shape[1]
    T = q.shape[1]
    k_all = jnp.concatenate([kv_buf[:, :, 0].astype(k_new.dtype), k_new], axis=1)
    v_all = jnp.concatenate([kv_buf[:, :, 1].astype(v_new.dtype), v_new], axis=1)
    idx = Lb + jnp.arange(T)[:, None] - dilation * jnp.arange(steps + 1)[None, :]
    valid = idx >= 0
    idx = jnp.maximum(idx, 0)
    kg = k_all[:, idx]
    vg = v_all[:, idx]
    s = jnp.einsum('bthd,btjhd->bthj', q, kg, preferred_element_type=F32) * (q.shape[-1] ** -0.5)
    p, lse = masked_softmax(s, valid[None, :, None, :])
    o = jnp.einsum('bthj,btjhd->bthd', p.astype(vg.dtype), vg)
    return o, lse


def dilated_mixer(xn, w_q, kv, w_out, kv_bufs):
    Bn, S, _ = xn.shape
    q = (xn @ w_q).reshape(Bn, S, N_GROUPS, ATTN_HEADS, ATTN_HEAD_DIM)
    outs, lses = [], []
    for g, (window, dilation) in enumerate(DILATED_GROUPS):
        steps = window // dilation
        if kv_bufs is None:
            o, lse = dilated_prompt(q[:, :, g], kv[:, :, g, 0], kv[:, :, g, 1], dilation, steps)
        else:
            o, lse = dilated_decode(q[:, :, g], kv[:, :, g, 0], kv[:, :, g, 1], kv_bufs[g], dilation, steps)
        outs.append(o.astype(F32))
        lses.append(lse)
    w = jax.nn.softmax(jnp.stack(lses), axis=0)[..., None]
    o = jnp.sum(w * jnp.stack(outs), axis=0).reshape(Bn, S, ATTN_WIDTH)
    return o.astype(xn.dtype) @ w_out


def trunk(x, C0, n0, m0, kv_bufs, norm_mix, norm_ffn, mlstm_w_in, mlstm_b_gates, mlstm_out_norm,
          mlstm_w_out, kv_norm, w_kv, attn_w_q, attn_w_out, ffn_w_gate, ffn_w_up, ffn_w_down,
          moe_w_router, moe_b_router, moe_w_gate, moe_w_up, moe_w_down, final_norm):
    Bn, S, _ = x.shape
    h = x
    Cs, ns, ms = [], [], []
    kv = None
    kv_rows = []
    for layer in range(DEPTH):
        xn = rmsnorm(h, norm_mix[layer])
        if layer < N_A_LAYERS:
            y, C, n, m = mlstm_mixer(xn, mlstm_w_in[layer], mlstm_b_gates[layer], mlstm_out_norm[layer],
                                     mlstm_w_out[layer], C0[layer], n0[layer], m0[layer])
            Cs.append(C)
            ns.append(n)
            ms.append(m)
        else:
            if kv is None:
                kv = (rmsnorm(h, kv_norm) @ w_kv).reshape(Bn, S, N_GROUPS, 2, ATTN_HEADS, ATTN_HEAD_DIM)
                for g, (window, _) in enumerate(DILATED_GROUPS):
                    keep = S if kv_bufs is not None else min(window, S)
                    kv_rows.append(kv[:, S - keep:, g])
            bi = layer - N_A_LAYERS
            y = dilated_mixer(xn, attn_w_q[bi], kv, attn_w_out[bi], kv_bufs)
        h = h + y
        xn = rmsnorm(h, norm_ffn[layer])
        j = layer // 2
        if layer % 2 == 0:
            f = swiglu(xn, ffn_w_gate[j], ffn_w_up[j], ffn_w_down[j])
        else:
            f = moe_swiglu(xn, moe_w_router[j], moe_b_router[j], moe_w_gate[j], moe_w_up[j], moe_w_down[j])
        h = h + f
    return (rmsnorm(h, final_norm), jnp.stack(Cs), jnp.stack(ns), jnp.stack(ms),
            kv_rows[0], kv_rows[1], kv_rows[2])


def setup_inputs(seed: int = 0) -> dict:
    key = jax.random.key(seed)
    keys = list(jax.random.split(key, 40))

    def nrm(i, shape, scale=1.0):
        return jax.random.normal(keys[i], shape, jnp.float32) * scale

    D = D_MODEL
    H = MLSTM_HEADS
    hv = MLSTM_HEADS * MLSTM_DV
    kv_shape = lambda w: (DEC_BATCH, min(w, PAST_LEN), 2, ATTN_HEADS, ATTN_HEAD_DIM)
    gate_bias = jnp.concatenate([nrm(12, (N_A_LAYERS, H), 0.1),
                                 3.0 + nrm(13, (N_A_LAYERS, H), 0.5)], axis=-1)
    return {
        'x_prompt': nrm(0, (BATCH, SEQ, D)),
        'x_sample': nrm(1, (DEC_BATCH, DEC_SEQ, D)),
        'state_mlstm_C': nrm(2, (N_A_LAYERS, DEC_BATCH, H, MLSTM_DK, MLSTM_DV), 0.05),
        'state_mlstm_n': nrm(3, (N_A_LAYERS, DEC_BATCH, H, MLSTM_DK), 0.05),
        'state_mlstm_m': nrm(4, (N_A_LAYERS, DEC_BATCH, H), 0.5),
        'cache_kv_w128': nrm(5, kv_shape(DILATED_GROUPS[0][0])),
        'cache_kv_w512': nrm(6, kv_shape(DILATED_GROUPS[1][0])),
        'cache_kv_w2048': nrm(7, kv_shape(DILATED_GROUPS[2][0])),
        'norm_mix': 1.0 + nrm(8, (DEPTH, D), 0.02),
        'norm_ffn': 1.0 + nrm(9, (DEPTH, D), 0.02),
        'mlstm_w_in': nrm(10, (N_A_LAYERS, D, MLSTM_PROJ), D ** -0.5),
        'mlstm_b_gates': gate_bias,
        'mlstm_out_norm': 1.0 + nrm(14, (N_A_LAYERS, hv), 0.02),
        'mlstm_w_out': nrm(15, (N_A_LAYERS, hv, D), hv ** -0.5),
        'kv_norm': 1.0 + nrm(16, (D,), 0.02),
        'w_kv': nrm(17, (D, N_GROUPS * 2 * ATTN_WIDTH), D ** -0.5),
        'attn_w_q': nrm(18, (N_B_LAYERS, D, N_GROUPS * ATTN_WIDTH), D ** -0.5),
        'attn_w_out': nrm(19, (N_B_LAYERS, ATTN_WIDTH, D), ATTN_WIDTH ** -0.5),
        'ffn_w_gate': nrm(20, (N_DENSE, D, D_FF), D ** -0.5),
        'ffn_w_up': nrm(21, (N_DENSE, D, D_FF), D ** -0.5),
        'ffn_w_down': nrm(22, (N_DENSE, D_FF, D), D_FF ** -0.5),
        'moe_w_router': nrm(23, (N_MOE, D, N_EXPERTS), D ** -0.5),
        'moe_b_router': nrm(24, (N_MOE, N_EXPERTS), 0.01),
        'moe_w_gate': nrm(25, (N_MOE, N_EXPERTS, D, D_FF_EXPERT), D ** -0.5),
        'moe_w_up': nrm(26, (N_MOE, N_EXPERTS, D, D_FF_EXPERT), D ** -0.5),
        'moe_w_down': nrm(27, (N_MOE, N_EXPERTS, D_FF_EXPERT, D), D_FF_EXPERT ** -0.5),
        'final_norm': 1.0 + nrm(28, (D,), 0.02),
    }


def reference(x_prompt, x_sample, state_mlstm_C, state_mlstm_n, state_mlstm_m, cache_kv_w128,
              cache_kv_w512, cache_kv_w2048, norm_mix, norm_ffn, mlstm_w_in, mlstm_b_gates,
              mlstm_out_norm, mlstm_w_out, kv_norm, w_kv, attn_w_q, attn_w_out, ffn_w_gate, ffn_w_up,
              ffn_w_down, moe_w_router, moe_b_router, moe_w_gate, moe_w_up, moe_w_down, final_norm):
    weights = (norm_mix, norm_ffn, mlstm_w_in, mlstm_b_gates, mlstm_out_norm, mlstm_w_out, kv_norm,
               w_kv, attn_w_q, attn_w_out, ffn_w_gate, ffn_w_up, ffn_w_down, moe_w_router,
               moe_b_router, moe_w_gate, moe_w_up, moe_w_down, final_norm)
    bp = x_prompt.shape[0]
    zero_C = jnp.zeros((N_A_LAYERS, bp, MLSTM_HEADS, MLSTM_DK, MLSTM_DV), F32)
    zero_n = jnp.zeros((N_A_LAYERS, bp, MLSTM_HEADS, MLSTM_DK), F32)
    zero_m = jnp.zeros((N_A_LAYERS, bp, MLSTM_HEADS), F32)
    y_prompt, p_C, p_n, p_m, p_kv128, p_kv512, p_kv2048 = trunk(
        x_prompt, zero_C, zero_n, zero_m, None, *weights)
    y_sample, s_C, s_n, s_m, s_kv128, s_kv512, s_kv2048 = trunk(
        x_sample, state_mlstm_C, state_mlstm_n, state_mlstm_m,
        (cache_kv_w128, cache_kv_w512, cache_kv_w2048), *weights)
    return (y_prompt, y_sample, p_C, p_n, p_m, p_kv128, p_kv512, p_kv2048,
            s_C, s_n, s_m, s_kv128, s_kv512, s_kv2048)
```

```python
from contextlib import ExitStack

import numpy as np
import concourse.bass as bass
import concourse.mybir as mybir
from concourse.bass_utils import run_bass_kernel_spmd

F32 = mybir.dt.float32
BF16 = mybir.dt.bfloat16
ALU = mybir.AluOpType
AF = mybir.ActivationFunctionType
AX = mybir.AxisListType
DS = bass.DynSlice

D = 2048
S = 2048
T = 2049
NH = 4
DK = 256
DV = 512
DFF = 5632
NE = 8
DFE = 2816
EPS = 1e-6
GROUPS = ((128, 1), (512, 4), (2048, 16))
NEG = -30000.0

TILES = [(i, i * 128, 128) for i in range(16)] + [(16, 2048, 1)]
BTILES = [(i, i * 512, 512) for i in range(4)] + [(4, 2048, 1)]


class Buf:
    __slots__ = ("w", "r")

    def __init__(self):
        self.w = None
        self.r = []


class Q:
    def __init__(self, T_, eng, name, is_pe=False):
        self.eng = eng
        self.name = name
        self.sem = T_.newsem(name)
        self.count = 0
        self.seen = {}
        self.is_pe = is_pe
        self.dsems = []
        self.dvals = []
        self.dnext = 0

    def wait(self, ev):
        if ev is None:
            return
        sem, val = ev
        if self.is_pe and sem is self.sem:
            return
        if self.seen.get(sem, 0) >= val:
            return
        self.eng.wait_ge(sem, val)
        self.seen[sem] = val


class _Stop(Exception):
    pass


class Tracker:
    limit = None
    dead = False

    def _tick(self, inc=True):
        if self.dead:
            return True
        if self.limit is not None and inc:
            if self.limit <= 0:
                self.dead = True
                return True
            self.limit -= 1
        return False

    def __init__(self, nc, es):
        self.nc = nc
        self.es = es
        self.bufs = {}
        self.pe = Q(self, nc.tensor, "pe", is_pe=True)
        self.act = Q(self, nc.scalar, "act")
        self.dve = Q(self, nc.vector, "dve")
        self.pool = Q(self, nc.gpsimd, "pool")
        self.sp = Q(self, nc.sync, "sp")
        self.qs = [self.pe, self.act, self.dve, self.pool, self.sp]
        for q, n in ((self.sp, 12), (self.pool, 8)):
            for i in range(n):
                q.dsems.append(self.newsem(f"{q.name}_d{i}"))
                q.dvals.append(0)

    def newsem(self, name):
        return self.es.enter_context(self.nc.semaphore(f"s_{name}"))

    def B(self, key):
        b = self.bufs.get(key)
        if b is None:
            b = self.bufs[key] = Buf()
        return b

    def _deps(self, q, reads, writes):
        for k in reads:
            q.wait(self.B(k).w)
        for k in writes:
            b = self.B(k)
            q.wait(b.w)
            for ev in b.r:
                q.wait(ev)

    def _commit(self, ev, reads, writes):
        for k in reads:
            b = self.B(k)
            b.r.append(ev)
            if len(b.r) > 32:
                b.r = b.r[-32:]
        for k in writes:
            b = self.B(k)
            b.w = ev
            b.r = []

    def op(self, q, fn, reads=(), writes=(), inc=True):
        if self._tick(inc):
            return None
        pr = [k for k in reads if isinstance(k, str) and k[0] == "p" and k[1] in "ABCT" and len(k) == 3]
        if pr:
            reads = [k for k in reads if k not in pr]
            writes = list(writes) + pr
        self._deps(q, reads, writes)
        ins = fn(q.eng)
        if inc:
            q.count += 1
            ins.then_inc(q.sem, 1)
            ev = (q.sem, q.count)
        else:
            ev = (q.sem, q.count + 1)
        self._commit(ev, reads, writes)
        return ins

    def dma(self, q, out, in_, reads=(), writes=(), slow=False):
        if self._tick():
            return None
        self._deps(q, reads, writes)
        i = q.dnext
        q.dnext = (q.dnext + 1) % len(q.dsems)
        sem = q.dsems[i]
        q.wait((sem, q.dvals[i]))
        ins = q.eng.dma_start(out=out, in_=in_, allow_slow_non_contiguous=True) if slow else q.eng.dma_start(out=out, in_=in_)
        q.dvals[i] += 16
        ins.then_inc(sem, 16)
        self._commit((sem, q.dvals[i]), reads, writes)
        return ins

    def barrier(self):
        evs = []
        for q in self.qs:
            if q.count:
                evs.append((q.sem, q.count))
            for s, v in zip(q.dsems, q.dvals):
                if v:
                    evs.append((s, v))
        for q in self.qs:
            for ev in evs:
                q.wait(ev)
        self.bufs = {}


def build_program(stop_after=99, only_attn=False, nheads=16, ngroups=3, dbg=0, limit=None):
    nc = bass.Bass("TRN2", target_bir_lowering=False)

    TINY = {"w_in", "w_out", "wg", "wu", "wd", "mwg", "mwu", "mwd", "wrT", "w_ao"} if only_attn else set()

    def din(name, shape, dt=F32):
        if name in TINY:
            shape = [1, 1]
        return nc.dram_tensor(name, list(shape), dt, kind="ExternalInput").ap()

    def dout(name, shape, dt=F32):
        return nc.dram_tensor(name, list(shape), dt, kind="ExternalOutput").ap()

    def dscr(name, shape, dt):
        return nc.dram_tensor(name, list(shape), dt).ap()

    xs = din("xs", [T, D])
    cst = din("cst", [128, 768])
    g_mix0 = din("g_mix0", [128, D])
    g_ffn0 = din("g_ffn0", [128, D])
    g_kv = din("g_kv", [128, D])
    g_mix1 = din("g_mix1", [128, D])
    g_ffn1 = din("g_ffn1", [128, D])
    g_fin = din("g_fin", [128, D])
    g_on = din("g_on", [128, D])
    w_in = din("w_in", [D, 6152])
    bgate = din("bgate", [1, 8])
    w_out = din("w_out", [D, D])
    w_kv = din("w_kv", [D, 12288])
    w_q = din("w_q", [D, 6144])
    w_ao = din("w_ao", [D, D])
    wg = din("wg", [D, DFF])
    wu = din("wu", [D, DFF])
    wd = din("wd", [DFF, D])
    wrT = din("wrT", [128, NE * D])
    brr = din("brr", [128, NE])
    mwg = din("mwg", [NE, D, DFE])
    mwu = din("mwu", [NE, D, DFE])
    mwd = din("mwd", [NE, DFE, D])
    C0 = din("C0", [NH, DK, DV])
    n0 = din("n0", [NH, DK])
    m0 = din("m0", [1, NH])
    caches = [din("ck128", [128, 2, 16, 128]), din("ck512", [512, 2, 16, 128]), din("ck2048", [2048, 2, 16, 128])]

    y = dout("y", [T, D])
    pC = dout("pC", [NH, DK, DV])
    pn = dout("pn", [NH, DK])
    pm = dout("pm", [1, NH])
    sC = dout("sC", [NH, DK, DV])
    sn = dout("sn", [NH, DK])
    sm = dout("sm", [1, NH])
    pkv = [dout("pkv0", [128, 2, 16, 128]), dout("pkv1", [512, 2, 16, 128]), dout("pkv2", [2048, 2, 16, 128])]
    skv = dout("skv", [3, 2, 16, 128])

    h = dscr("h_res", [T, D], F32)
    hT = dscr("hT", [D, T], BF16)
    qTd = dscr("qTd", [3 * D, T], BF16)
    oT = dscr("oT", [D, T], BF16)

    with ExitStack() as es0:
        Tk = Tracker(nc, es0)
        pe, act, dve, pool, sp = Tk.pe, Tk.act, Tk.dve, Tk.pool, Tk.sp
        op, dma = Tk.op, Tk.dma

        def sbt(es, name, shape, dt):
            return es.enter_context(nc.sbuf_tensor(name, list(shape), dt))

        pA = [es0.enter_context(nc.psum_tensor(f"pA{i}", [128, 512], F32)) for i in range(2)]
        pBk = [es0.enter_context(nc.psum_tensor(f"pB{i}", [128, 512], F32)) for i in range(2)]
        pCk = [es0.enter_context(nc.psum_tensor(f"pC{i}", [128, 512], F32)) for i in range(2)]
        pTk = [es0.enter_context(nc.psum_tensor(f"pT{i}", [128, 8, 128], BF16)) for i in range(2)]

        cf = sbt(es0, "cf", [128, 768], F32)
        idb = sbt(es0, "idb", [128, 128], BF16)
        onb = sbt(es0, "onb", [128, 128], BF16)
        mcur_b = sbt(es0, "mcur_b", [128, 128], BF16)
        mprev_b = sbt(es0, "mprev_b", [128, 128], BF16)
        dma(sp, cf[:], cst, writes=["cf"])
        op(dve, lambda e: e.tensor_copy(idb[:], cf[:, 0:128]), reads=["cf"], writes=["idb"])
        op(dve, lambda e: e.tensor_copy(onb[:], cf[:, 128:256]), reads=["cf"], writes=["onb"])
        op(dve, lambda e: e.tensor_copy(mcur_b[:], cf[:, 256:384]), reads=["cf"], writes=["mcur_b"])
        op(dve, lambda e: e.tensor_copy(mprev_b[:], cf[:, 384:512]), reads=["cf"], writes=["mprev_b"])
        onf = cf[:, 128:256]
        mcur_f = cf[:, 512:640]
        CONST = ["cf", "idb", "onb", "mcur_b", "mprev_b"]

        def const_reset():
            pass

        rr = {"ev": 0}

        def evac_eng():
            rr["ev"] += 1
            return act if rr["ev"] % 2 else dve

        def copy_op(q, out, in_, reads, writes):
            if q is act:
                return op(act, lambda e: e.copy(out, in_), reads=reads, writes=writes)
            return op(q, lambda e: e.tensor_copy(out, in_), reads=reads, writes=writes)

        def norm_stage(es, src, dsts, router=None, tag="n"):
            xin = [sbt(es, f"{tag}_xin{i}", [128, D], F32) for i in range(2)]
            junk = sbt(es, f"{tag}_junk", [128, D], BF16)
            st = sbt(es, f"{tag}_st", [128, 4], F32)
            gains = []
            for j, (gd, _, _) in enumerate(dsts):
                gt = sbt(es, f"{tag}_g{j}", [128, D], F32)
                dma(sp, gt[:], gd, writes=[f"{tag}_g{j}"])
                gains.append(gt)
            xnb = [sbt(es, f"{tag}_xnb{i}", [128, D], BF16) for i in range(2)]
            if router is not None:
                xnf = sbt(es, f"{tag}_xnf", [128, D], F32)
                jf = sbt(es, f"{tag}_jf", [128, D], F32)
                wr_sb = sbt(es, f"{tag}_wr", [128, NE * D], F32)
                br_sb = sbt(es, f"{tag}_br", [128, NE], F32)
                lg = sbt(es, f"{tag}_lg", [128, NE], F32)
                rt = sbt(es, f"{tag}_rt", [128, 4 * NE], F32)
                dma(sp, wr_sb[:], wrT, writes=["wr_sb"])
                dma(sp, br_sb[:], brr, writes=["br_sb"])
                gates_sb = router
            cnt = 0
            for (ti, t0, nt) in TILES:
                x_ = xin[ti % 2]
                xk = f"{tag}_xin{ti % 2}"
                dma(sp, x_[0:nt, :], src[t0:t0 + nt, :], writes=[xk])
                op(act, lambda e: e.activation(junk[0:nt, :], x_[0:nt, :], AF.Square, accum_out=st[0:nt, 0:1]),
                   reads=[xk], writes=[f"{tag}_junk", f"{tag}_st"])
                op(dve, lambda e: e.tensor_scalar(st[0:nt, 1:2], st[0:nt, 0:1], 1.0 / D, EPS, ALU.mult, ALU.add),
                   reads=[f"{tag}_st"], writes=[f"{tag}_st"])
                op(act, lambda e: e.activation(st[0:nt, 2:3], st[0:nt, 1:2], AF.Sqrt), reads=[f"{tag}_st"], writes=[f"{tag}_st"])
                op(dve, lambda e: e.reciprocal(st[0:nt, 3:4], st[0:nt, 2:3]), reads=[f"{tag}_st"], writes=[f"{tag}_st"])
                for j, (_, dstT, dkey) in enumerate(dsts):
                    xb = xnb[cnt % 2]
                    bk = f"{tag}_xnb{cnt % 2}"
                    cnt += 1
                    if router is not None:
                        op(dve, lambda e: e.scalar_tensor_tensor(xnf[0:nt, :], x_[0:nt, :], st[0:nt, 3:4], gains[j][0:nt, :], ALU.mult, ALU.mult),
                           reads=[xk, f"{tag}_st", f"{tag}_g{j}"], writes=["xnf"])
                        op(act, lambda e: e.copy(xb[0:nt, :], xnf[0:nt, :]), reads=["xnf"], writes=[bk])
                    else:
                        op(dve, lambda e: e.scalar_tensor_tensor(xb[0:nt, :], x_[0:nt, :], st[0:nt, 3:4], gains[j][0:nt, :], ALU.mult, ALU.mult),
                           reads=[xk, f"{tag}_st", f"{tag}_g{j}"], writes=[bk])
                    for half in range(2):
                        pt = pTk[half]
                        pk = f"pT{half}"
                        for c in range(8):
                            cc = half * 8 + c
                            op(pe, lambda e: e.transpose(pt[:, c, 0:nt], xb[0:nt, cc * 128:(cc + 1) * 128], idb[0:nt, 0:nt]),
                               reads=[bk, "idb"], writes=[pk], inc=(c == 7))
                        copy_op(evac_eng(), dstT[:, half * 8:(half + 1) * 8, t0:t0 + nt], pt[:, :, 0:nt], [pk], [dkey])
                if router is not None:
                    for e_ in range(NE):
                        op(dve, lambda e: e.tensor_tensor(jf[0:nt, :], xnf[0:nt, :], wr_sb[0:nt, e_ * D:(e_ + 1) * D], ALU.mult),
                           reads=["xnf", "wr_sb"], writes=["jf"])
                        op(dve, lambda e: e.reduce_sum(lg[0:nt, e_:e_ + 1], jf[0:nt, :], AX.X), reads=["jf"], writes=["lg"])
                    op(dve, lambda e: e.tensor_tensor(lg[0:nt, :], lg[0:nt, :], br_sb[0:nt, :], ALU.add), reads=["lg", "br_sb"], writes=["lg"])
                    op(dve, lambda e: e.reduce_max(rt[0:nt, 0:1], lg[0:nt, :], AX.X), reads=["lg"], writes=["rt"])
                    op(dve, lambda e: e.tensor_scalar(rt[0:nt, 8:16], lg[0:nt, :], rt[0:nt, 0:1], -1e30, ALU.is_equal, ALU.mult), reads=["lg", "rt"], writes=["rt"])
                    op(dve, lambda e: e.tensor_tensor(rt[0:nt, 8:16], rt[0:nt, 8:16], lg[0:nt, :], ALU.add), reads=["lg", "rt"], writes=["rt"])
                    op(dve, lambda e: e.reduce_max(rt[0:nt, 1:2], rt[0:nt, 8:16], AX.X), reads=["rt"], writes=["rt"])
                    op(dve, lambda e: e.tensor_scalar(rt[0:nt, 16:24], lg[0:nt, :], rt[0:nt, 1:2], None, ALU.is_ge), reads=["lg", "rt"], writes=["rt"])
                    op(dve, lambda e: e.tensor_scalar(rt[0:nt, 2:3], rt[0:nt, 0:1], -1.0, None, ALU.mult), reads=["rt"], writes=["rt"])
                    op(act, lambda e: e.activation(rt[0:nt, 24:32], lg[0:nt, :], AF.Exp, bias=rt[0:nt, 2:3]), reads=["lg", "rt"], writes=["rt"])
                    op(dve, lambda e: e.tensor_tensor(rt[0:nt, 24:32], rt[0:nt, 24:32], rt[0:nt, 16:24], ALU.mult), reads=["rt"], writes=["rt"])
                    op(dve, lambda e: e.reduce_sum(rt[0:nt, 3:4], rt[0:nt, 24:32], AX.X), reads=["rt"], writes=["rt"])
                    op(dve, lambda e: e.reciprocal(rt[0:nt, 4:5], rt[0:nt, 3:4]), reads=["rt"], writes=["rt"])
                    op(dve, lambda e: e.tensor_scalar(gates_sb[0:nt, ti, :], rt[0:nt, 24:32], rt[0:nt, 4:5], None, ALU.mult), reads=["rt"], writes=["gates"])

        def outproj(es, aT, akey, KC, W_ap, res_in, res_out, cbw, gates_sb=None, gcol=None, tag="o"):
            ncb = D // cbw
            wb = [sbt(es, f"{tag}_wb{i}", [128, KC, cbw], BF16) for i in range(2)]
            rin = [sbt(es, f"{tag}_rin{i}", [128, cbw], F32) for i in range(3)]
            Wv = W_ap.rearrange("(c p) m -> p c m", p=128)
            k = 0
            for cb in range(ncb):
                w_ = wb[cb % 2]
                wk_ = f"{tag}_wb{cb % 2}"
                dma(pool, w_[:], Wv[:, :, cb * cbw:(cb + 1) * cbw], writes=[wk_])
                for (ti, t0, nt) in TILES:
                    pp = pA[k % 2]
                    pk = f"pA{k % 2}"
                    r_ = rin[k % 3]
                    rk = f"{tag}_rin{k % 3}"
                    k += 1
                    dma(sp, r_[0:nt, :], res_in[t0:t0 + nt, cb * cbw:(cb + 1) * cbw], reads=[("hres", ti, cb)] if res_in is h else [], writes=[rk])
                    for kc in range(KC):
                        op(pe, lambda e: e.matmul(pp[0:nt, 0:cbw], aT[:, kc, t0:t0 + nt], w_[:, kc, :], start=(kc == 0), stop=(kc == KC - 1)),
                           reads=[akey, wk_], writes=[pk], inc=(kc == KC - 1))
                    if gates_sb is None:
                        op(dve, lambda e: e.tensor_tensor(r_[0:nt, :], r_[0:nt, :], pp[0:nt, 0:cbw], ALU.add), reads=[pk, rk], writes=[rk])
                    else:
                        op(dve, lambda e: e.scalar_tensor_tensor(r_[0:nt, :], pp[0:nt, 0:cbw], gates_sb[0:nt, ti, gcol:gcol + 1], r_[0:nt, :], ALU.mult, ALU.add),
                           reads=[pk, rk, "gates"], writes=[rk])
                    dma(sp, res_out[t0:t0 + nt, cb * cbw:(cb + 1) * cbw], r_[0:nt, :], reads=[rk], writes=[("hres", ti, cb)])

        def ffn_block(es, xT, xkey, Wg_ap, Wu_ap, Wd_ap, gates_sb=None, gcol=None, tag="f"):
            NMC = DFE // 128
            actT = sbt(es, f"{tag}_actT", [128, NMC, T], BF16)
            wgc = [sbt(es, f"{tag}_wg{i}", [128, 16, 128], BF16) for i in range(2)]
            wuc = [sbt(es, f"{tag}_wu{i}", [128, 16, 128], BF16) for i in range(2)]
            sg = [sbt(es, f"{tag}_sg{i}", [128, 512], F32) for i in range(2)]
            Wgv = Wg_ap.rearrange("(c p) m -> p c m", p=128)
            Wuv = Wu_ap.rearrange("(c p) m -> p c m", p=128)
            k = 0
            for mc in range(NMC):
                g_ = wgc[mc % 2]
                u_ = wuc[mc % 2]
                gk = f"{tag}_wg{mc % 2}"
                uk = f"{tag}_wu{mc % 2}"
                dma(pool, g_[:], Wgv[:, :, mc * 128:(mc + 1) * 128], writes=[gk])
                dma(pool, u_[:], Wuv[:, :, mc * 128:(mc + 1) * 128], writes=[uk])
                for (bi, t0, nt) in BTILES:
                    pg = pBk[k % 2]
                    pu = pCk[k % 2]
                    pgk = f"pB{k % 2}"
                    puk = f"pC{k % 2}"
                    s_ = sg[k % 2]
                    sk = f"{tag}_sg{k % 2}"
                    k += 1
                    for kc in range(16):
                        op(pe, lambda e: e.matmul(pg[:, 0:nt], g_[:, kc, :], xT[:, kc, t0:t0 + nt], start=(kc == 0), stop=(kc == 15)),
                           reads=[gk, xkey], writes=[pgk], inc=(kc == 15))
                    for kc in range(16):
                        op(pe, lambda e: e.matmul(pu[:, 0:nt], u_[:, kc, :], xT[:, kc, t0:t0 + nt], start=(kc == 0), stop=(kc == 15)),
                           reads=[uk, xkey], writes=[puk], inc=(kc == 15))
                    op(act, lambda e: e.activation(s_[:, 0:nt], pg[:, 0:nt], AF.Silu), reads=[pgk], writes=[sk])
                    op(dve, lambda e: e.tensor_tensor(actT[:, mc, t0:t0 + nt], s_[:, 0:nt], pu[:, 0:nt], ALU.mult), reads=[sk, puk], writes=[f"{tag}_actT"])
            outproj(es, actT, f"{tag}_actT", NMC, Wd_ap, h, h, 256, gates_sb=gates_sb, gcol=gcol, tag=tag + "d")

        with ExitStack() as esA:
            if not only_attn:
              xn0T = sbt(esA, "xn0T", [128, 16, T], BF16)
              with ExitStack() as es:
                norm_stage(es, xs, [(g_mix0, xn0T, "xn0T")], tag="n0")
                Tk.barrier()
            if stop_after >= 2 and not only_attn:
              with ExitStack() as es:
                Wh = sbt(es, "Wh", [128, 16, 1536], BF16)
                Wig = sbt(es, "Wig", [128, 16, 128], BF16)
                Wlf = sbt(es, "Wlf", [128, 16, 128], BF16)
                R = [sbt(es, f"R{i}", [128, T], F32) for i in range(4)]
                Z = sbt(es, "Zrow", [128, T], F32)
                bcol = sbt(es, "bcol", [128, 4], F32)
                mfin = sbt(es, "mfin", [128, 4], F32)
                gon = sbt(es, "gon", [128, D], F32)
                Cf = sbt(es, "Cf", [128, 2, 512], F32)
                Cb = sbt(es, "Cb", [128, 2, 512], BF16)
                nf = sbt(es, "nf", [128, 2], F32)
                nb = sbt(es, "nb", [128, 2], BF16)
                w_inv = w_in.rearrange("(c p) m -> p c m", p=128)
                dma(sp, gon[:], g_on, writes=["gon"])
                qk = [sbt(es, f"qk{i}", [128, 512], BF16) for i in range(2)]
                qw = [sbt(es, f"qw{i}", [128, 256], BF16) for i in range(2)]
                vs_ = [sbt(es, f"vs{i}", [128, 512], BF16) for i in range(2)]
                so = [sbt(es, f"so{i}", [128, 512], F32) for i in range(2)]
                cols = [sbt(es, f"cols{i}", [128, 8], F32) for i in range(2)]
                qkT = [sbt(es, f"qkT{i}", [128, 6, 128], BF16) for i in range(2)]
                tmpf = sbt(es, "tmpf", [128, 128], F32)
                WT = sbt(es, "WT", [128, 128], F32)
                STb = sbt(es, "STb", [128, 128], BF16)
                kw = sbt(es, "kw", [128, 256], BF16)
                hf = sbt(es, "hf", [128, 512], F32)
                hj = sbt(es, "hj", [128, 512], BF16)
                hg = sbt(es, "hg", [128, 512], BF16)
                hst = sbt(es, "hst", [128, 8], F32)
                hTs = [sbt(es, f"hTs{i}", [128, 4, 128], BF16) for i in range(2)]
                hTv = hT.rearrange("(c p) t -> p c t", p=128)
                itn = 0
                for hgrp in ([0, 1, 2], [3]):
                    op(pool, lambda e: e.memset(Wig[:], 0.0), writes=["Wig"])
                    op(pool, lambda e: e.memset(Wlf[:], 0.0), writes=["Wlf"])
                    op(dve, lambda e: e.memset(bcol[:], 0.0), writes=["bcol"])
                    op(dve, lambda e: e.memset(Z[:], 0.0), writes=["Z"])
                    for i in range(4):
                        op(dve, lambda e: e.memset(R[i][:], 0.0), writes=[f"R{i}"])
                    gbase = 2 * NH * DK + 2 * NH * DV
                    for hh in hgrp:
                        dma(pool, Wig[:, :, 32 * hgrp.index(hh):32 * hgrp.index(hh) + 1], w_inv[:, :, gbase + hh:gbase + hh + 1], writes=["Wig"], slow=True)
                        dma(pool, Wlf[:, :, 32 * hgrp.index(hh):32 * hgrp.index(hh) + 1], w_inv[:, :, gbase + NH + hh:gbase + NH + hh + 1], writes=["Wlf"], slow=True)
                        dma(sp, bcol[32 * hgrp.index(hh):32 * hgrp.index(hh) + 1, 0:1], bgate[0:1, hh:hh + 1], writes=["bcol"])
                        dma(sp, bcol[32 * hgrp.index(hh):32 * hgrp.index(hh) + 1, 1:2], bgate[0:1, NH + hh:NH + hh + 1], writes=["bcol"])
                        dma(sp, bcol[32 * hgrp.index(hh):32 * hgrp.index(hh) + 1, 2:3], m0[0:1, hh:hh + 1], writes=["bcol"])
                    op(dve, lambda e: e.tensor_scalar(bcol[:, 0:2], bcol[:, 0:2], 1.0 / 15.0, None, ALU.mult), reads=["bcol"], writes=["bcol"])
                    for (bi, t0, nt) in BTILES:
                        for (Wt, wk_, pp, pk, col, Rd, rk) in ((Wig, "Wig", pA[0], "pA0", 0, R[0], "R0"), (Wlf, "Wlf", pA[1], "pA1", 1, R[1], "R1")):
                            for kc in range(16):
                                op(pe, lambda e: e.matmul(pp[:, 0:nt], Wt[:, kc, :], xn0T[:, kc, t0:t0 + nt], start=(kc == 0), stop=(kc == 15)),
                                   reads=[wk_, "xn0T"], writes=[pk], inc=(kc == 15))
                            op(act, lambda e: e.activation(Rd[:, t0:t0 + nt], pp[:, 0:nt], AF.Tanh, bias=bcol[:, col:col + 1], scale=1.0 / 15.0),
                               reads=[pk, "bcol"], writes=[rk])
                    op(dve, lambda e: e.tensor_scalar(R[0][:], R[0][:], 15.0, None, ALU.mult), reads=["R0"], writes=["R0"])
                    op(act, lambda e: e.activation(R[1][:], R[1][:], AF.Exp, scale=-15.0), reads=["R1"], writes=["R1"])
                    op(act, lambda e: e.activation(R[1][:], R[1][:], AF.Ln, bias=1.0), reads=["R1"], writes=["R1"])
                    op(dve, lambda e: e.tensor_scalar(R[1][:], R[1][:], -1.0, None, ALU.mult), reads=["R1"], writes=["R1"])
                    op(dve, lambda e: e.tensor_tensor_scan(R[2][:, 0:S], R[1][:, 0:S], Z[:, 0:S], 0.0, ALU.add, ALU.add), reads=["R1", "Z"], writes=["R2"])
                    op(dve, lambda e: e.tensor_copy(R[2][:, S:T], R[1][:, S:T]), reads=["R1"], writes=["R2"])
                    op(dve, lambda e: e.tensor_tensor_scan(R[3][:, 0:S], R[1][:, 0:S], R[0][:, 0:S], 0.0, ALU.add, ALU.max), reads=["R1", "R0"], writes=["R3"])
                    op(dve, lambda e: e.scalar_tensor_tensor(R[3][:, S:T], R[1][:, S:T], bcol[:, 2:3], R[0][:, S:T], ALU.add, ALU.max), reads=["R1", "R0", "bcol"], writes=["R3"])
                    op(dve, lambda e: e.tensor_copy(mfin[:, 0:1], R[3][:, S - 1:S]), reads=["R3"], writes=["mfin"])
                    op(dve, lambda e: e.tensor_copy(mfin[:, 1:2], R[3][:, S:T]), reads=["R3"], writes=["mfin"])
                    op(dve, lambda e: e.tensor_tensor(R[1][:], R[2][:], R[3][:], ALU.subtract), reads=["R2", "R3"], writes=["R1"])
                    op(dve, lambda e: e.tensor_tensor(R[2][:], R[0][:], R[2][:], ALU.subtract), reads=["R0", "R2"], writes=["R2"])
                    op(act, lambda e: e.activation(R[3][:], R[3][:], AF.Exp, scale=-1.0), reads=["R3"], writes=["R3"])
                    op(dve, lambda e: e.tensor_copy(R[0][:, 0:128], R[1][:, 0:128]), reads=["R1"], writes=["R0"])
                    for i in range(1, 16):
                        op(dve, lambda e: e.tensor_scalar(R[0][:, i * 128:(i + 1) * 128], R[1][:, i * 128:(i + 1) * 128], R[1][:, i * 128 - 1:i * 128], None, ALU.subtract),
                           reads=["R1"], writes=["R0"])
                    op(dve, lambda e: e.tensor_scalar(R[0][:, S:T], R[1][:, S:T], bcol[:, 2:3], None, ALU.add), reads=["R1", "bcol"], writes=["R0"])
                    op(act, lambda e: e.activation(R[0][:], R[0][:], AF.Exp), reads=["R0"], writes=["R0"])
                    Rwi, Ru, Rc, Rem = R[0], R[1], R[2], R[3]
                    for hh in hgrp:
                        dma(sp, pm[0:1, hh:hh + 1], mfin[32 * hgrp.index(hh):32 * hgrp.index(hh) + 1, 0:1], reads=["mfin"], writes=["o_pm"])
                        dma(sp, sm[0:1, hh:hh + 1], mfin[32 * hgrp.index(hh):32 * hgrp.index(hh) + 1, 1:2], reads=["mfin"], writes=["o_sm"])

                    for hh in hgrp:
                        r0 = 32 * hgrp.index(hh)
                        segs = ((hh * DK, 0, DK), (NH * DK + hh * DK, DK, DK), (2 * NH * DK + hh * DV, 2 * DK, DV), (2 * NH * DK + NH * DV + hh * DV, 2 * DK + DV, DV))
                        for (src0, dst0, wdt) in segs:
                            dma(pool, Wh[:, :, dst0:dst0 + wdt], w_inv[:, :, src0:src0 + wdt], writes=["Wh"])
                        op(dve, lambda e: e.memset(Cf[:], 0.0), writes=["Cf"])
                        op(pool, lambda e: e.memset(Cb[:], 0.0), writes=["Cb"])
                        op(dve, lambda e: e.memset(nf[:], 0.0), writes=["nf"])
                        op(pool, lambda e: e.memset(nb[:], 0.0), writes=["nb"])
                        for (ti, t0, nt) in TILES:
                            if ti == 16:
                                dma(sp, pC[hh].rearrange("(c p) e -> p c e", p=128), Cf[:], reads=["Cf"], writes=["o_pC"])
                                for dc in range(2):
                                    dma(sp, pn[hh:hh + 1, dc * 128:(dc + 1) * 128].rearrange("o p -> p o"), nf[:, dc:dc + 1], reads=["nf"], writes=["o_pn"], slow=True)
                                dma(sp, Cf[:], C0[hh].rearrange("(c p) e -> p c e", p=128), writes=["Cf"])
                                for dc in range(2):
                                    dma(sp, nf[:, dc:dc + 1], n0[hh:hh + 1, dc * 128:(dc + 1) * 128].rearrange("o p -> p o"), writes=["nf"], slow=True)
                                op(act, lambda e: e.copy(Cb[:], Cf[:]), reads=["Cf"], writes=["Cb"])
                                op(dve, lambda e: e.tensor_copy(nb[:], nf[:]), reads=["nf"], writes=["nb"])
                            sl = itn % 2
                            itn += 1
                            qk_, qw_, v_, so_, cl_, qT_ = qk[sl], qw[sl], vs_[sl], so[sl], cols[sl], qkT[sl]
                            kq, kqw, kv_, kso, kcl, kqT = f"qk{sl}", f"qw{sl}", f"vs{sl}", f"so{sl}", f"cols{sl}", f"qkT{sl}"
                            pq, pv, po_ = pA[sl], pBk[sl], pCk[sl]
                            kpq, kpv, kpo = f"pA{sl}", f"pB{sl}", f"pC{sl}"
                            for (pp, pk, c0) in ((pq, kpq, 0), (pv, kpv, 512), (po_, kpo, 1024)):
                                for kc in range(16):
                                    op(pe, lambda e: e.matmul(pp[0:nt, :], xn0T[:, kc, t0:t0 + nt], Wh[:, kc, c0:c0 + 512], start=(kc == 0), stop=(kc == 15)),
                                       reads=["xn0T", "Wh"], writes=[pk], inc=(kc == 15))
                            pD = pTk
                            psm = pCk[1 - sl] if False else None
                            px = pCk[1 - sl]
                            kpx = f"pC{1 - sl}"
                            for j, Rr in enumerate((Rc, Rwi, Rem)):
                                rkey = ("R2", "R0", "R3")[j]
                                op(pe, lambda e: e.matmul(px[0:nt, j:j + 1], Rr[r0:r0 + 1, t0:t0 + nt], onf[r0:r0 + 1, 0:1], start=True, stop=True),
                                   reads=[rkey, "cf"], writes=[kpx], inc=False)
                            op(pe, lambda e: e.matmul(px[:, 4:5], onf[r0:r0 + 1, 0:128], Rwi[r0:r0 + 1, t0 + nt - 1:t0 + nt], start=True, stop=True),
                               reads=["R0", "cf"], writes=[kpx], inc=False)
                            op(pe, lambda e: e.matmul(px[0:nt, 128:128 + nt], onf[r0:r0 + 1, 0:nt], Ru[r0:r0 + 1, t0:t0 + nt], start=True, stop=True),
                               reads=["R1", "cf"], writes=[kpx])
                            op(act, lambda e: e.copy(cl_[0:nt, 0:3], px[0:nt, 0:3]), reads=[kpx], writes=[kcl])
                            op(act, lambda e: e.copy(cl_[:, 4:5], px[:, 4:5]), reads=[kpx], writes=[kcl])
                            op(dve, lambda e: e.tensor_tensor(tmpf[0:nt, 0:nt], px[0:nt, 128:128 + nt], mcur_f[0:nt, 0:nt], ALU.add), reads=[kpx, "cf"], writes=["tmpf"])
                            op(act, lambda e: e.activation(WT[0:nt, 0:nt], tmpf[0:nt, 0:nt], AF.Exp, bias=cl_[0:nt, 0:1]), reads=["tmpf", kcl], writes=["WT"])
                            op(act, lambda e: e.copy(qk_[0:nt, 0:256], pq[0:nt, 0:256]), reads=[kpq], writes=[kq])
                            op(dve, lambda e: e.tensor_scalar(qk_[0:nt, 256:512], pq[0:nt, 256:512], DK ** -0.5, None, ALU.mult), reads=[kpq], writes=[kq])
                            op(dve, lambda e: e.tensor_scalar(qw_[0:nt, :], pq[0:nt, 0:256], cl_[0:nt, 1:2], None, ALU.mult), reads=[kpq, kcl], writes=[kqw])
                            op(act, lambda e: e.copy(v_[0:nt, :], pv[0:nt, :]), reads=[kpv], writes=[kv_])
                            op(act, lambda e: e.activation(so_[0:nt, :], po_[0:nt, :], AF.Sigmoid), reads=[kpo], writes=[kso])
                            pt = pTk[sl]
                            kpt = f"pT{sl}"
                            srcs = ((qk_, kq, 0), (qk_, kq, 128), (qw_, kqw, 0), (qw_, kqw, 128), (qk_, kq, 256), (qk_, kq, 384))
                            for j, (sv, skk, c0) in enumerate(srcs):
                                op(pe, lambda e: e.transpose(pt[:, j, 0:nt], sv[0:nt, c0:c0 + 128], idb[0:nt, 0:nt]), reads=[skk, "idb"], writes=[kpt], inc=(j == 5))
                            op(dve, lambda e: e.tensor_copy(qT_[:, :, 0:nt], pt[:, 0:6, 0:nt]), reads=[kpt], writes=[kqT])
                            for dc in range(2):
                                op(pe, lambda e: e.matmul(px[0:nt, 256:256 + nt], qT_[:, 4 + dc, 0:nt], qT_[:, dc, 0:nt], start=(dc == 0), stop=(dc == 1)),
                                   reads=[kqT], writes=[kpx], inc=(dc == 1))
                            op(dve, lambda e: e.tensor_tensor(STb[0:nt, 0:nt], px[0:nt, 256:256 + nt], WT[0:nt, 0:nt], ALU.mult), reads=[kpx, "WT"], writes=["STb"])
                            op(dve, lambda e: e.tensor_scalar(kw[0:nt, :], qk_[0:nt, 256:512], WT[0:nt, nt - 1:nt], None, ALU.mult), reads=[kq, "WT"], writes=["kw"])
                            pnum = pq
                            op(pe, lambda e: e.matmul(pnum[0:nt, :], STb[0:nt, 0:nt], v_[0:nt, :], start=True, stop=False), reads=["STb", kv_, kq, kqw], writes=[kpq], inc=False)
                            for dc in range(2):
                                op(pe, lambda e: e.matmul(pnum[0:nt, :], qT_[:, 2 + dc, 0:nt], Cb[:, dc, :], start=False, stop=(dc == 1)), reads=[kqT, "Cb"], writes=[kpq], inc=(dc == 1))
                            op(pe, lambda e: e.matmul(px[0:nt, 8:9], STb[0:nt, 0:nt], onb[0:nt, 0:1], start=True, stop=False), reads=["STb", "onb"], writes=[kpx], inc=False)
                            for dc in range(2):
                                op(pe, lambda e: e.matmul(px[0:nt, 8:9], qT_[:, 2 + dc, 0:nt], nb[:, dc:dc + 1], start=False, stop=(dc == 1)), reads=[kqT, "nb"], writes=[kpx], inc=(dc == 1))
                            op(act, lambda e: e.activation(hst[0:nt, 6:7], px[0:nt, 8:9], AF.Abs), reads=[kpx], writes=["hst"])
                            op(dve, lambda e: e.tensor_tensor(hst[0:nt, 0:1], hst[0:nt, 6:7], cl_[0:nt, 2:3], ALU.max), reads=["hst", kcl], writes=["hst"])
                            op(dve, lambda e: e.reciprocal(hst[0:nt, 1:2], hst[0:nt, 0:1]), reads=["hst"], writes=["hst"])
                            op(dve, lambda e: e.tensor_scalar(hf[0:nt, :], pnum[0:nt, :], hst[0:nt, 1:2], None, ALU.mult), reads=[kpq, "hst"], writes=["hf"])
                            for dc in range(2):
                                pcs = (pv, po_)[dc]
                                kpcs = (kpv, kpo)[dc]
                                op(pe, lambda e: e.matmul(pcs[:, :], kw[0:nt, dc * 128:(dc + 1) * 128], v_[0:nt, :], start=True, stop=True), reads=["kw", kv_, kso], writes=[kpcs])
                                op(pe, lambda e: e.matmul(px[:, 12 + dc:13 + dc], kw[0:nt, dc * 128:(dc + 1) * 128], onb[0:nt, 0:1], start=True, stop=True), reads=["kw", "onb"], writes=[kpx])
                                op(dve, lambda e: e.scalar_tensor_tensor(Cf[:, dc, :], Cf[:, dc, :], cl_[:, 4:5], pcs[:, :], ALU.mult, ALU.add), reads=[kpcs, kcl, "Cf"], writes=["Cf"])
                            op(dve, lambda e: e.scalar_tensor_tensor(nf[:, :], nf[:, :], cl_[:, 4:5], px[:, 12:14], ALU.mult, ALU.add), reads=[kpx, kcl, "nf"], writes=["nf"])
                            op(act, lambda e: e.copy(Cb[:], Cf[:]), reads=["Cf"], writes=["Cb"])
                            op(dve, lambda e: e.tensor_copy(nb[:], nf[:]), reads=["nf"], writes=["nb"])
                            op(act, lambda e: e.activation(hj[0:nt, :], hf[0:nt, :], AF.Square, accum_out=hst[0:nt, 2:3]), reads=["hf"], writes=["hj", "hst"])
                            op(dve, lambda e: e.tensor_scalar(hst[0:nt, 3:4], hst[0:nt, 2:3], 1.0 / DV, EPS, ALU.mult, ALU.add), reads=["hst"], writes=["hst"])
                            op(act, lambda e: e.activation(hst[0:nt, 4:5], hst[0:nt, 3:4], AF.Sqrt), reads=["hst"], writes=["hst"])
                            op(dve, lambda e: e.reciprocal(hst[0:nt, 5:6], hst[0:nt, 4:5]), reads=["hst"], writes=["hst"])
                            op(dve, lambda e: e.scalar_tensor_tensor(hf[0:nt, :], hf[0:nt, :], hst[0:nt, 5:6], gon[0:nt, hh * DV:(hh + 1) * DV], ALU.mult, ALU.mult), reads=["hf", "hst", "gon"], writes=["hf"])
                            op(dve, lambda e: e.tensor_tensor(hg[0:nt, :], hf[0:nt, :], so_[0:nt, :], ALU.mult), reads=["hf", kso], writes=["hg"])
                            for j in range(4):
                                op(pe, lambda e: e.transpose(pt[:, j, 0:nt], hg[0:nt, j * 128:(j + 1) * 128], idb[0:nt, 0:nt]), reads=["hg", "idb", kqT], writes=[kpt], inc=(j == 3))
                            ht_ = hTs[sl]
                            kht = f"hTs{sl}"
                            op(act, lambda e: e.copy(ht_[:, :, 0:nt], pt[:, 0:4, 0:nt]), reads=[kpt], writes=[kht])
                            dma(sp, hTv[:, hh * 4:(hh + 1) * 4, t0:t0 + nt], ht_[:, :, 0:nt], reads=[kht], writes=[("hT", hh, ti)], slow=(nt == 1))
                        dma(sp, sC[hh].rearrange("(c p) e -> p c e", p=128), Cf[:], reads=["Cf"], writes=["o_sC"])
                        for dc in range(2):
                            dma(sp, sn[hh:hh + 1, dc * 128:(dc + 1) * 128].rearrange("o p -> p o"), nf[:, dc:dc + 1], reads=["nf"], writes=["o_sn"], slow=True)
                Tk.barrier()

        if stop_after >= 3 and not only_attn:
            with ExitStack() as es:
                aT = sbt(es, "aT", [128, 16, T], BF16)
                dma(sp, aT[:], hT.rearrange("(c p) t -> p c t", p=128), writes=["aT"])
                outproj(es, aT, "aT", 16, w_out, xs, h, 512, tag="wo")
                Tk.barrier()
        if stop_after >= 5 and not only_attn:
            with ExitStack() as esA:
                xn1T = sbt(esA, "xn1T", [128, 16, T], BF16)
                with ExitStack() as es:
                    norm_stage(es, h, [(g_ffn0, xn1T, "xn1T")], tag="n1")
                    Tk.barrier()
                for fb in range(2):
                    with ExitStack() as es:
                        ffn_block(es, xn1T, "xn1T", wg[:, fb * DFE:(fb + 1) * DFE], wu[:, fb * DFE:(fb + 1) * DFE], wd[fb * DFE:(fb + 1) * DFE, :], tag=f"f{fb}")
                        Tk.barrier()
        if stop_after >= 8:
            with ExitStack() as esA:
                xkvT = sbt(esA, "xkvT", [128, 16, T], BF16)
                with ExitStack() as esB:
                    xqT = sbt(esB, "xqT", [128, 16, T], BF16)
                    with ExitStack() as es:
                        norm_stage(es, xs if only_attn else h, [(g_kv, xkvT, "xkvT"), (g_mix1, xqT, "xqT")], tag="n2")
                        Tk.barrier()
                    with ExitStack() as es:
                        wqc = [sbt(es, f"wqc{i}", [128, 16, 128], BF16) for i in range(2)]
                        qTs = [sbt(es, f"qTs{i}", [128, T], BF16) for i in range(2)]
                        w_qv = w_q.rearrange("(c p) m -> p c m", p=128)
                        k = 0
                        for mc in range(48):
                            w_ = wqc[mc % 2]
                            wk_ = f"wqc{mc % 2}"
                            q_ = qTs[mc % 2]
                            qk_ = f"qTs{mc % 2}"
                            dma(pool, w_[:], w_qv[:, :, mc * 128:(mc + 1) * 128], writes=[wk_])
                            for (bi, t0, nt) in BTILES:
                                pp = pA[k % 2]
                                pk = f"pA{k % 2}"
                                k += 1
                                for kc in range(16):
                                    op(pe, lambda e: e.matmul(pp[:, 0:nt], w_[:, kc, :], xqT[:, kc, t0:t0 + nt], start=(kc == 0), stop=(kc == 15)),
                                       reads=[wk_, "xqT"], writes=[pk], inc=(kc == 15))
                                copy_op(evac_eng(), q_[:, t0:t0 + nt], pp[:, 0:nt], [pk], [qk_])
                            dma(sp, qTd[mc * 128:(mc + 1) * 128, :], q_[:], reads=[qk_], writes=[("qTd", mc)])
                        Tk.barrier()
                with ExitStack() as es:
                    Oacc = sbt(es, "Oacc", [128, T], F32)
                    Lacc = sbt(es, "Lacc", [128, T], F32)
                    oTh = sbt(es, "oTh", [128, T], BF16)
                    wkv = [sbt(es, f"wkv{i}", [128, 16, 256], BF16) for i in range(2)]
                    qTh = [sbt(es, f"qTh{i}", [128, T], BF16) for i in range(2)]
                    KT = sbt(es, "KT", [128, 16, 128], BF16)
                    Vall = sbt(es, "Vall", [128, 16, 128], BF16)
                    kvf = [sbt(es, f"kvf{i}", [128, 256], F32) for i in range(3)]
                    kvb = [sbt(es, f"kvb{i}", [128, 128], BF16) for i in range(2)]
                    cKV = sbt(es, "cKV", [128, 2, 128], BF16)
                    cKT = sbt(es, "cKT", [128, 128], BF16)
                    KsT = sbt(es, "KsT", [128, 1], BF16)
                    Vs = sbt(es, "Vs", [1, 128], BF16)
                    Pb = [sbt(es, f"Pb{i}", [128, 2, 128], BF16) for i in range(2)]
                    w_kvv = w_kv.rearrange("(c p) m -> p c m", p=128)
                    scale = 128.0 ** -0.5
                    it = 0
                    fcount = 0
                    Tk.limit = limit
                    for hd in range(nheads):
                        op(pool, lambda e: e.memset(Oacc[:], 0.0), writes=["Oacc"])
                        op(pool, lambda e: e.memset(Lacc[:], 0.0), writes=["Lacc"])
                        for g, (window, r) in enumerate(GROUPS[:ngroups]):
                            nbk = 16 // r
                            sl = it % 2
                            it += 1
                            wk_ = wkv[sl]
                            kwk = f"wkv{sl}"
                            q_ = qTh[sl]
                            kq_ = f"qTh{sl}"
                            ck = ((g * 2 + 0) * 16 + hd) * 128
                            cv = ((g * 2 + 1) * 16 + hd) * 128
                            dma(pool, wk_[:, :, 0:128], w_kvv[:, :, ck:ck + 128], writes=[kwk])
                            dma(pool, wk_[:, :, 128:256], w_kvv[:, :, cv:cv + 128], writes=[kwk])
                            dma(sp, q_[:], qTd[(g * 16 + hd) * 128:(g * 16 + hd + 1) * 128, :], reads=[("qTd", g * 16 + hd)], writes=[kq_])
                            if dbg != 12:
                                dma(pool, cKV[:], caches[g][DS(0, 128, r), :, hd, :], writes=["cKV"])
                            for f in range(17 if dbg != 13 else 16):
                                if f < 16:
                                    res, blk = f // nbk, f % nbk
                                    start = res + r * 128 * blk
                                    nt = 128
                                    tok = DS(start, 128, r)
                                else:
                                    nt = 1
                                    tok = DS(S, 1, 1)
                                pp = pA[fcount % 2]
                                pk = f"pA{fcount % 2}"
                                kf_ = kvf[fcount % 3]
                                kkf = f"kvf{fcount % 3}"
                                kb_ = kvb[fcount % 2]
                                kkb = f"kvb{fcount % 2}"
                                pt = pTk[fcount % 2]
                                kpt = f"pT{fcount % 2}"
                                fcount += 1
                                for kc in range(16):
                                    op(pe, lambda e: e.matmul(pp[0:nt, 0:256], xkvT[:, kc, tok], wk_[:, kc, :], start=(kc == 0), stop=(kc == 15)),
                                       reads=["xkvT", kwk], writes=[pk], inc=(kc == 15))
                                op(act, lambda e: e.copy(kf_[0:nt, :], pp[0:nt, 0:256]), reads=[pk], writes=[kkf])
                                op(dve, lambda e: e.tensor_copy(kb_[0:nt, :], pp[0:nt, 0:128]), reads=[pk], writes=[kkb])
                                if f < 16:
                                    op(dve, lambda e: e.tensor_copy(Vall[:, f, :], pp[:, 128:256]), reads=[pk], writes=[("Vall", f)])
                                    op(pe, lambda e: e.transpose(pt[:, 0, :], kb_[:, :], idb[:]), reads=[kkb, "idb"], writes=[kpt])
                                    op(act, lambda e: e.copy(KT[:, f, :], pt[:, 0, :]), reads=[kpt], writes=[("KT", f)])
                                    kview = kf_[:].rearrange("p (a d) -> p a d", a=2)
                                    if dbg == 11:
                                        pass
                                    elif g == 0 and f == 15:
                                        dma(sp, pkv[0][:, :, hd, :], kview, reads=[kkf], writes=["o_pkv"])
                                    elif g == 1 and blk == 3:
                                        dma(sp, pkv[1][DS(res, 128, 4), :, hd, :], kview, reads=[kkf], writes=["o_pkv"])
                                    elif g == 2:
                                        dma(sp, pkv[2][DS(res, 128, 16), :, hd, :], kview, reads=[kkf], writes=["o_pkv"])
                                else:
                                    op(dve, lambda e: e.tensor_copy(Vs[0:1, :], pp[0:1, 128:256]), reads=[pk], writes=["Vs"])
                                    op(pe, lambda e: e.transpose(pt[:, 0, 0:1], kb_[0:1, :], idb[0:1, 0:1]), reads=[kkb, "idb"], writes=[kpt])
                                    op(act, lambda e: e.copy(KsT[:, 0:1], pt[:, 0, 0:1]), reads=[kpt], writes=["KsT"])
                                    if dbg != 11:
                                        dma(sp, skv[g, :, hd, :], kf_[0:1, :].rearrange("p (a d) -> p a d", a=2), reads=[kkf], writes=["o_skv"])
                            if dbg in (1, 11, 12, 13, 14):
                                continue
                            ptc = pTk[fcount % 2]
                            kptc = f"pT{fcount % 2}"
                            op(pe, lambda e: e.transpose(ptc[:, 1, :], cKV[:, 0, :], idb[:]), reads=["cKV", "idb"], writes=[kptc])
                            op(act, lambda e: e.copy(cKT[:], ptc[:, 1, :]), reads=[kptc], writes=["cKT"])
                            for f in range(16 if dbg != 3 else 0):
                                res, blk = f // nbk, f % nbk
                                start = res + r * 128 * blk
                                qv = q_[:, DS(start, 128, r)]
                                keys = ([(f - 1, mprev_b, "mprev_b")] if blk > 0 else []) + [(f, mcur_b, "mcur_b")]
                                ps_ = pBk[f % 2]
                                kps = f"pB{f % 2}"
                                po2 = pCk[f % 2]
                                kpo2 = f"pC{f % 2}"
                                P_ = Pb[f % 2]
                                kP = f"Pb{f % 2}"
                                nk = len(keys)
                                for s_i, (kfi, mk, mkk) in enumerate(keys):
                                    op(pe, lambda e: e.matmul(ps_[:, s_i * 128:(s_i + 1) * 128], KT[:, kfi, :], qv, start=True, stop=False),
                                       reads=[("KT", kfi), kq_], writes=[kps], inc=False)
                                    op(pe, lambda e: e.matmul(ps_[:, s_i * 128:(s_i + 1) * 128], idb[:], mk[:], start=False, stop=True),
                                       reads=["idb", mkk], writes=[kps], inc=(s_i == nk - 1))
                                op(act, lambda e: e.activation(P_[:, 0:nk, :], ps_[:, 0:nk * 128].rearrange("p (a q) -> p a q", a=nk), AF.Exp, scale=scale),
                                   reads=[kps], writes=[kP])
                                for s_i, (kfi, mk, mkk) in enumerate(keys):
                                    op(pe, lambda e: e.matmul(po2[:, 0:128], Vall[:, kfi, :], P_[:, s_i, :], start=(s_i == 0), stop=(s_i == nk - 1)),
                                       reads=[("Vall", kfi), kP], writes=[kpo2], inc=False)
                                for s_i, (kfi, mk, mkk) in enumerate(keys):
                                    op(pe, lambda e: e.matmul(po2[:, 128:256], onb[:], P_[:, s_i, :], start=(s_i == 0), stop=(s_i == nk - 1)),
                                       reads=["onb", kP], writes=[kpo2], inc=(s_i == nk - 1))
                                op(dve, lambda e: e.tensor_tensor(Oacc[:, DS(start, 128, r)], Oacc[:, DS(start, 128, r)], po2[:, 0:128], ALU.add), reads=[kpo2, "Oacc"], writes=["Oacc"])
                                op(dve, lambda e: e.tensor_tensor(Lacc[:, DS(start, 128, r)], Lacc[:, DS(start, 128, r)], po2[:, 128:256], ALU.add), reads=[kpo2, "Lacc"], writes=["Lacc"])
                            if dbg == 2:
                                continue
                            ps_ = pBk[0]
                            po2 = pCk[0]
                            P_ = Pb[0]
                            qs_ = q_[:, S:T]
                            op(pe, lambda e: e.matmul(ps_[:, 0:1], cKT[:], qs_, start=True, stop=True), reads=["cKT", kq_], writes=["pB0"])
                            op(pe, lambda e: e.matmul(ps_[0:1, 128:129], KsT[:, 0:1], qs_, start=True, stop=True), reads=["KsT", kq_], writes=["pB0"])
                            op(act, lambda e: e.activation(P_[:, 0, 0:1], ps_[:, 0:1], AF.Exp, scale=scale), reads=["pB0"], writes=["Pb0"])
                            op(act, lambda e: e.activation(P_[0:1, 1, 0:1], ps_[0:1, 128:129], AF.Exp, scale=scale), reads=["pB0"], writes=["Pb0"])
                            op(pe, lambda e: e.matmul(po2[:, 0:1], cKV[:, 1, :], P_[:, 0, 0:1], start=True, stop=False), reads=["cKV", "Pb0"], writes=["pC0"], inc=False)
                            op(pe, lambda e: e.matmul(po2[:, 0:1], Vs[0:1, :], P_[0:1, 1, 0:1], start=False, stop=True), reads=["Vs", "Pb0"], writes=["pC0"], inc=False)
                            op(pe, lambda e: e.matmul(po2[:, 128:129], onb[:], P_[:, 0, 0:1], start=True, stop=False), reads=["onb", "Pb0"], writes=["pC0"], inc=False)
                            op(pe, lambda e: e.matmul(po2[:, 128:129], onb[0:1, :], P_[0:1, 1, 0:1], start=False, stop=True), reads=["onb", "Pb0"], writes=["pC0"])
                            op(dve, lambda e: e.tensor_tensor(Oacc[:, S:T], Oacc[:, S:T], po2[:, 0:1], ALU.add), reads=["pC0", "Oacc"], writes=["Oacc"])
                            op(dve, lambda e: e.tensor_tensor(Lacc[:, S:T], Lacc[:, S:T], po2[:, 128:129], ALU.add), reads=["pC0", "Lacc"], writes=["Lacc"])
                        op(dve, lambda e: e.reciprocal(Lacc[:], Lacc[:]), reads=["Lacc"], writes=["Lacc"])
                        op(dve, lambda e: e.tensor_tensor(oTh[:], Oacc[:], Lacc[:], ALU.mult), reads=["Oacc", "Lacc"], writes=["oTh"])
                        dma(sp, oT[hd * 128:(hd + 1) * 128, :], oTh[:], reads=["oTh"], writes=[("oT", hd)])
                    Tk.limit = None
                    Tk.dead = False
                    Tk.barrier()
        if stop_after >= 9:
            with ExitStack() as es:
                aT = sbt(es, "aT2", [128, 16, T], BF16)
                dma(sp, aT[:], oT.rearrange("(c p) t -> p c t", p=128), writes=["aT2"])
                outproj(es, aT, "aT2", 16, w_ao, h, h, 512, tag="ao")
                Tk.barrier()
        if stop_after >= 11:
            with ExitStack() as esA:
                xn3T = sbt(esA, "xn3T", [128, 16, T], BF16)
                gates_sb = sbt(esA, "gates", [128, 17, NE], F32)
                with ExitStack() as es:
                    norm_stage(es, h, [(g_ffn1, xn3T, "xn3T")], router=gates_sb, tag="n3")
                    Tk.barrier()
                for e_ in range(NE):
                    with ExitStack() as es:
                        ffn_block(es, xn3T, "xn3T", mwg[e_], mwu[e_], mwd[e_], gates_sb=gates_sb, gcol=e_, tag=f"m{e_}")
                        Tk.barrier()
        with ExitStack() as es:
            xin = [sbt(es, f"fx{i}", [128, D], F32) for i in range(2)]
            yo = [sbt(es, f"fy{i}", [128, D], F32) for i in range(2)]
            junk = sbt(es, "fjunk", [128, D], BF16)
            st = sbt(es, "fst", [128, 4], F32)
            gt = sbt(es, "fg", [128, D], F32)
            dma(sp, gt[:], g_fin, writes=["fg"])
            src = h if (stop_after >= 3 and not only_attn) else xs
            for (ti, t0, nt) in TILES:
                x_ = xin[ti % 2]
                xk = f"fx{ti % 2}"
                y_ = yo[ti % 2]
                yk = f"fy{ti % 2}"
                dma(sp, x_[0:nt, :], src[t0:t0 + nt, :], writes=[xk])
                op(act, lambda e: e.activation(junk[0:nt, :], x_[0:nt, :], AF.Square, accum_out=st[0:nt, 0:1]), reads=[xk], writes=["fjunk", "fst"])
                op(dve, lambda e: e.tensor_scalar(st[0:nt, 1:2], st[0:nt, 0:1], 1.0 / D, EPS, ALU.mult, ALU.add), reads=["fst"], writes=["fst"])
                op(act, lambda e: e.activation(st[0:nt, 2:3], st[0:nt, 1:2], AF.Sqrt), reads=["fst"], writes=["fst"])
                op(dve, lambda e: e.reciprocal(st[0:nt, 3:4], st[0:nt, 2:3]), reads=["fst"], writes=["fst"])
                op(dve, lambda e: e.scalar_tensor_tensor(y_[0:nt, :], x_[0:nt, :], st[0:nt, 3:4], gt[0:nt, :], ALU.mult, ALU.mult), reads=[xk, "fst", "fg"], writes=[yk])
                dma(sp, y[t0:t0 + nt, :], y_[0:nt, :], reads=[yk], writes=["o_y"])
            Tk.barrier()
    return nc


def _consts():
    c = np.zeros((128, 768), np.float32)
    c[:, 0:128] = np.eye(128, dtype=np.float32)
    c[:, 128:256] = 1.0
    k = np.arange(128)[:, None]
    q = np.arange(128)[None, :]
    c[:, 256:384] = np.where(k <= q, 0.0, NEG)
    c[:, 384:512] = np.where(k >= q, 0.0, NEG)
    c[:, 512:640] = np.where(k <= q, 0.0, -1e30)
    return c


_NC_CACHE = {}


def kernel(x_prompt, x_sample, state_mlstm_C, state_mlstm_n, state_mlstm_m, cache_kv_w128, cache_kv_w512,
           cache_kv_w2048, norm_mix, norm_ffn, mlstm_w_in, mlstm_b_gates, mlstm_out_norm, mlstm_w_out, kv_norm,
           w_kv, attn_w_q, attn_w_out, ffn_w_gate, ffn_w_up, ffn_w_down, moe_w_router, moe_b_router, moe_w_gate,
           moe_w_up, moe_w_down, final_norm, _stop_after=99):
    f = lambda a: np.ascontiguousarray(np.asarray(a, dtype=np.float32))
    rep = lambda v: np.ascontiguousarray(np.broadcast_to(np.asarray(v, np.float32).reshape(1, -1), (128, np.asarray(v).size)))
    if _stop_after not in _NC_CACHE:
        _NC_CACHE[_stop_after] = build_program(_stop_after)
    nc = _NC_CACHE[_stop_after]
    shared = {
        "cst": _consts(),
        "g_mix0": rep(norm_mix[0]), "g_ffn0": rep(norm_ffn[0]), "g_kv": rep(kv_norm), "g_mix1": rep(norm_mix[1]),
        "g_ffn1": rep(norm_ffn[1]), "g_fin": rep(final_norm), "g_on": rep(mlstm_out_norm[0]),
        "w_in": f(mlstm_w_in[0]), "bgate": f(mlstm_b_gates).reshape(1, 8), "w_out": f(mlstm_w_out[0]), "w_kv": f(w_kv),
        "w_q": f(attn_w_q[0]), "w_ao": f(attn_w_out[0]), "wg": f(ffn_w_gate[0]), "wu": f(ffn_w_up[0]), "wd": f(ffn_w_down[0]),
        "wrT": rep(np.asarray(moe_w_router[0], np.float32).T.reshape(-1)), "brr": rep(moe_b_router[0]),
        "mwg": f(moe_w_gate[0]), "mwu": f(moe_w_up[0]), "mwd": f(moe_w_down[0]),
    }
    xp = np.asarray(x_prompt, np.float32)
    xsm = np.asarray(x_sample, np.float32)
    in_maps = []
    for c in range(8):
        m = dict(shared)
        m["xs"] = np.ascontiguousarray(np.concatenate([xp[c % 4], xsm[c]], axis=0))
        m["C0"] = f(state_mlstm_C[0, c])
        m["n0"] = f(state_mlstm_n[0, c])
        m["m0"] = f(state_mlstm_m[0, c]).reshape(1, 4)
        m["ck128"] = f(cache_kv_w128[c])
        m["ck512"] = f(cache_kv_w512[c])
        m["ck2048"] = f(cache_kv_w2048[c])
        in_maps.append(m)
    res = run_bass_kernel_spmd(nc, in_maps, core_ids=list(range(8))).results
    g = lambda c, n: np.asarray(res[c][n], dtype=np.float32)
    y_prompt = np.stack([g(b, "y")[:S] for b in range(4)])
    y_sample = np.stack([g(c, "y")[S:T] for c in range(8)])
    p_C = np.stack([g(b, "pC") for b in range(4)])[None]
    p_n = np.stack([g(b, "pn") for b in range(4)])[None]
    p_m = np.stack([g(b, "pm")[0] for b in range(4)])[None]
    p_kv = [np.stack([g(b, f"pkv{i}") for b in range(4)]) for i in range(3)]
    s_C = np.stack([g(c, "sC") for c in range(8)])[None]
    s_n = np.stack([g(c, "sn") for c in range(8)])[None]
    s_m = np.stack([g(c, "sm")[0] for c in range(8)])[None]
    s_kv = [np.stack([g(c, "skv")[i][None] for c in range(8)]) for i in range(3)]
    return (y_prompt, y_sample, p_C, p_n, p_m, p_kv[0], p_kv[1], p_kv[2], s_C, s_n, s_m, s_kv[0], s_kv[1], s_kv[2])
```

```python
from contextlib import ExitStack

import numpy as np
import concourse.bass as bass
import concourse.mybir as mybir
from concourse.bass_utils import run_bass_kernel_spmd

F32 = mybir.dt.float32
BF16 = mybir.dt.bfloat16
ALU = mybir.AluOpType
AF = mybir.ActivationFunctionType
AX = mybir.AxisListType
DS = bass.DynSlice

D = 2048
S = 2048
T = 2049
NH = 4
DK = 256
DV = 512
DFF = 5632
NE = 8
DFE = 2816
EPS = 1e-6
GROUPS = ((128, 1), (512, 4), (2048, 16))
NEG = -30000.0

TM = 1025
TILES = [(i, i * 128, 128) for i in range(16)] + [(16, 2048, 1)]
MTILES = [(i, i * 128, 128) for i in range(8)] + [(8, 1024, 1)]
MBTILES = [(i, i * 512, 512) for i in range(2)] + [(2, 1024, 1)]
BTILES = [(i, i * 512, 512) for i in range(4)] + [(4, 2048, 1)]


class Buf:
    __slots__ = ("w", "r")

    def __init__(self):
        self.w = None
        self.r = []


class Q:
    def __init__(self, T_, eng, name, is_pe=False):
        self.eng = eng
        self.name = name
        self.sem = T_.newsem(name)
        self.count = 0
        self.seen = {}
        self.is_pe = is_pe
        self.dsems = []
        self.dvals = []
        self.dnext = 0

    def wait(self, ev):
        if ev is None:
            return
        sem, val = ev
        if self.is_pe and sem is self.sem:
            return
        if self.seen.get(sem, 0) >= val:
            return
        self.eng.wait_ge(sem, val)
        self.seen[sem] = val


class _Stop(Exception):
    pass


class Tracker:
    limit = None
    dead = False

    def _tick(self, inc=True):
        if self.dead:
            return True
        if self.limit is not None and inc:
            if self.limit <= 0:
                self.dead = True
                return True
            self.limit -= 1
        return False

    def __init__(self, nc, es):
        self.nc = nc
        self.es = es
        self.bufs = {}
        self.pe = Q(self, nc.tensor, "pe", is_pe=True)
        self.act = Q(self, nc.scalar, "act")
        self.dve = Q(self, nc.vector, "dve")
        self.pool = Q(self, nc.gpsimd, "pool")
        self.sp = Q(self, nc.sync, "sp")
        self.qs = [self.pe, self.act, self.dve, self.pool, self.sp]
        for q, n in ((self.sp, 12), (self.pool, 8)):
            for i in range(n):
                q.dsems.append(self.newsem(f"{q.name}_d{i}"))
                q.dvals.append(0)

    def newsem(self, name):
        return self.es.enter_context(self.nc.semaphore(f"s_{name}"))

    def B(self, key):
        b = self.bufs.get(key)
        if b is None:
            b = self.bufs[key] = Buf()
        return b

    def _deps(self, q, reads, writes):
        for k in reads:
            q.wait(self.B(k).w)
        for k in writes:
            b = self.B(k)
            q.wait(b.w)
            for ev in b.r:
                q.wait(ev)

    def _commit(self, ev, reads, writes):
        for k in reads:
            b = self.B(k)
            b.r.append(ev)
            if len(b.r) > 32:
                b.r = b.r[-32:]
        for k in writes:
            b = self.B(k)
            b.w = ev
            b.r = []

    def op(self, q, fn, reads=(), writes=(), inc=True):
        if self._tick(inc):
            return None
        pr = [k for k in reads if isinstance(k, str) and k[0] == "p" and k[1] in "ABCT" and len(k) == 3]
        if pr:
            reads = [k for k in reads if k not in pr]
            writes = list(writes) + pr
        self._deps(q, reads, writes)
        ins = fn(q.eng)
        if inc:
            q.count += 1
            ins.then_inc(q.sem, 1)
            ev = (q.sem, q.count)
        else:
            ev = (q.sem, q.count + 1)
        self._commit(ev, reads, writes)
        return ins

    def dma(self, q, out, in_, reads=(), writes=(), slow=False):
        if self._tick():
            return None
        self._deps(q, reads, writes)
        i = q.dnext
        q.dnext = (q.dnext + 1) % len(q.dsems)
        sem = q.dsems[i]
        q.wait((sem, q.dvals[i]))
        ins = q.eng.dma_start(out=out, in_=in_, allow_slow_non_contiguous=True) if slow else q.eng.dma_start(out=out, in_=in_)
        q.dvals[i] += 16
        ins.then_inc(sem, 16)
        self._commit((sem, q.dvals[i]), reads, writes)
        return ins

    def barrier(self):
        evs = []
        for q in self.qs:
            if q.count:
                evs.append((q.sem, q.count))
            for s, v in zip(q.dsems, q.dvals):
                if v:
                    evs.append((s, v))
        for q in self.qs:
            for ev in evs:
                q.wait(ev)
        self.bufs = {}


def build_program(stop_after=99, only_attn=False, nheads=16, ngroups=3, dbg=0, limit=None):
    nc = bass.Bass("TRN2", target_bir_lowering=False)

    TINY = {"w_in", "w_out", "wg", "wu", "wd", "mwg", "mwu", "mwd", "wrT", "w_ao"} if only_attn else set()

    def din(name, shape, dt=F32):
        if name in TINY:
            shape = [1, 1]
        return nc.dram_tensor(name, list(shape), dt, kind="ExternalInput").ap()

    def dout(name, shape, dt=F32):
        return nc.dram_tensor(name, list(shape), dt, kind="ExternalOutput").ap()

    def dscr(name, shape, dt):
        return nc.dram_tensor(name, list(shape), dt).ap()

    xs = din("xs", [T, D])
    cst = din("cst", [128, 768])
    selv = din("selv", [128, 2])
    g_mix0 = din("g_mix0", [128, D])
    g_ffn0 = din("g_ffn0", [128, D])
    g_kv = din("g_kv", [128, D])
    g_mix1 = din("g_mix1", [128, D])
    g_ffn1 = din("g_ffn1", [128, D])
    g_fin = din("g_fin", [128, D])
    g_on = din("g_on", [128, D])
    w_in = din("w_in", [D, 6152])
    bgate = din("bgate", [1, 8])
    w_out = din("w_out", [D, D])
    w_kv = din("w_kv", [D, 12288])
    w_q = din("w_q", [D, 6144])
    w_ao = din("w_ao", [D, D])
    wg = din("wg", [D, DFF])
    wu = din("wu", [D, DFF])
    wd = din("wd", [DFF, D])
    wrT = din("wrT", [128, NE * D])
    brr = din("brr", [128, NE])
    mwg = din("mwg", [NE, D, DFE])
    mwu = din("mwu", [NE, D, DFE])
    mwd = din("mwd", [NE, DFE, D])
    C0 = din("C0", [NH, DK, DV])
    n0 = din("n0", [NH, DK])
    m0 = din("m0", [1, NH])
    caches = [din("ck128", [128, 2, 16, 128]), din("ck512", [512, 2, 16, 128]), din("ck2048", [2048, 2, 16, 128])]

    y = dout("y", [TM, D])
    pC = dout("pC", [NH, DK, DV])
    pn = dout("pn", [NH, DK])
    pm = dout("pm", [1, NH])
    sC = dout("sC", [NH, DK, DV])
    sn = dout("sn", [NH, DK])
    sm = dout("sm", [1, NH])
    pkv = [dout("pkv0", [128, 2, 16, 128]), dout("pkv1", [512, 2, 16, 128]), dout("pkv2", [2048, 2, 16, 128])]
    skv = dout("skv", [3, 2, 16, 128])

    h = dscr("h_res", [T, D], F32)
    hT = dscr("hT", [D, T], BF16)
    qTd = dscr("qTd", [3 * D, T], BF16)
    oT = dscr("oT", [D, T], BF16)
    hm = dscr("hm", [TM, D], F32)

    with ExitStack() as es0:
        Tk = Tracker(nc, es0)
        pe, act, dve, pool, sp = Tk.pe, Tk.act, Tk.dve, Tk.pool, Tk.sp
        op, dma = Tk.op, Tk.dma

        def sbt(es, name, shape, dt):
            return es.enter_context(nc.sbuf_tensor(name, list(shape), dt))

        pA = [es0.enter_context(nc.psum_tensor(f"pA{i}", [128, 512], F32)) for i in range(2)]
        pBk = [es0.enter_context(nc.psum_tensor(f"pB{i}", [128, 512], F32)) for i in range(2)]
        pCk = [es0.enter_context(nc.psum_tensor(f"pC{i}", [128, 512], F32)) for i in range(2)]
        pTk = [es0.enter_context(nc.psum_tensor(f"pT{i}", [128, 8, 128], BF16)) for i in range(2)]

        cf = sbt(es0, "cf", [128, 768], F32)
        idb = sbt(es0, "idb", [128, 128], BF16)
        onb = sbt(es0, "onb", [128, 128], BF16)
        mcur_b = sbt(es0, "mcur_b", [128, 128], BF16)
        mprev_b = sbt(es0, "mprev_b", [128, 128], BF16)
        dma(sp, cf[:], cst, writes=["cf"])
        op(dve, lambda e: e.tensor_copy(idb[:], cf[:, 0:128]), reads=["cf"], writes=["idb"])
        op(dve, lambda e: e.tensor_copy(onb[:], cf[:, 128:256]), reads=["cf"], writes=["onb"])
        op(dve, lambda e: e.tensor_copy(mcur_b[:], cf[:, 256:384]), reads=["cf"], writes=["mcur_b"])
        op(dve, lambda e: e.tensor_copy(mprev_b[:], cf[:, 384:512]), reads=["cf"], writes=["mprev_b"])
        onf = cf[:, 128:256]
        mcur_f = cf[:, 512:640]
        CONST = ["cf", "idb", "onb", "mcur_b", "mprev_b"]

        def const_reset():
            pass

        rr = {"ev": 0}

        def evac_eng():
            rr["ev"] += 1
            return act if rr["ev"] % 2 else dve

        def copy_op(q, out, in_, reads, writes):
            if q is act:
                return op(act, lambda e: e.copy(out, in_), reads=reads, writes=writes)
            return op(q, lambda e: e.tensor_copy(out, in_), reads=reads, writes=writes)

        def norm_stage(es, src, dsts, router=None, tag="n", tiles=None):
            xin = [sbt(es, f"{tag}_xin{i}", [128, D], F32) for i in range(2)]
            junk = sbt(es, f"{tag}_junk", [128, D], BF16)
            st = sbt(es, f"{tag}_st", [128, 4], F32)
            gains = []
            for j, (gd, _, _) in enumerate(dsts):
                gt = sbt(es, f"{tag}_g{j}", [128, D], F32)
                dma(sp, gt[:], gd, writes=[f"{tag}_g{j}"])
                gains.append(gt)
            xnb = [sbt(es, f"{tag}_xnb{i}", [128, D], BF16) for i in range(2)]
            if router is not None:
                xnf = sbt(es, f"{tag}_xnf", [128, D], F32)
                jf = sbt(es, f"{tag}_jf", [128, D], F32)
                wr_sb = sbt(es, f"{tag}_wr", [128, NE * D], F32)
                br_sb = sbt(es, f"{tag}_br", [128, NE], F32)
                lg = sbt(es, f"{tag}_lg", [128, NE], F32)
                rt = sbt(es, f"{tag}_rt", [128, 4 * NE], F32)
                dma(sp, wr_sb[:], wrT, writes=["wr_sb"])
                dma(sp, br_sb[:], brr, writes=["br_sb"])
                gates_sb = router
            cnt = 0
            for (ti, t0, nt) in (tiles or TILES):
                x_ = xin[ti % 2]
                xk = f"{tag}_xin{ti % 2}"
                dma(sp, x_[0:nt, :], src[t0:t0 + nt, :], writes=[xk])
                op(act, lambda e: e.activation(junk[0:nt, :], x_[0:nt, :], AF.Square, accum_out=st[0:nt, 0:1]),
                   reads=[xk], writes=[f"{tag}_junk", f"{tag}_st"])
                op(dve, lambda e: e.tensor_scalar(st[0:nt, 1:2], st[0:nt, 0:1], 1.0 / D, EPS, ALU.mult, ALU.add),
                   reads=[f"{tag}_st"], writes=[f"{tag}_st"])
                op(act, lambda e: e.activation(st[0:nt, 2:3], st[0:nt, 1:2], AF.Sqrt), reads=[f"{tag}_st"], writes=[f"{tag}_st"])
                op(dve, lambda e: e.reciprocal(st[0:nt, 3:4], st[0:nt, 2:3]), reads=[f"{tag}_st"], writes=[f"{tag}_st"])
                for j, (_, dstT, dkey) in enumerate(dsts):
                    xb = xnb[cnt % 2]
                    bk = f"{tag}_xnb{cnt % 2}"
                    cnt += 1
                    if router is not None:
                        op(dve, lambda e: e.scalar_tensor_tensor(xnf[0:nt, :], x_[0:nt, :], st[0:nt, 3:4], gains[j][0:nt, :], ALU.mult, ALU.mult),
                           reads=[xk, f"{tag}_st", f"{tag}_g{j}"], writes=["xnf"])
                        op(act, lambda e: e.copy(xb[0:nt, :], xnf[0:nt, :]), reads=["xnf"], writes=[bk])
                    else:
                        op(dve, lambda e: e.scalar_tensor_tensor(xb[0:nt, :], x_[0:nt, :], st[0:nt, 3:4], gains[j][0:nt, :], ALU.mult, ALU.mult),
                           reads=[xk, f"{tag}_st", f"{tag}_g{j}"], writes=[bk])
                    for half in range(2):
                        pt = pTk[half]
                        pk = f"pT{half}"
                        for c in range(8):
                            cc = half * 8 + c
                            op(pe, lambda e: e.transpose(pt[:, c, 0:nt], xb[0:nt, cc * 128:(cc + 1) * 128], idb[0:nt, 0:nt]),
                               reads=[bk, "idb"], writes=[pk], inc=(c == 7))
                        copy_op(evac_eng(), dstT[:, half * 8:(half + 1) * 8, t0:t0 + nt], pt[:, :, 0:nt], [pk], [dkey])
                if router is not None:
                    for e_ in range(NE):
                        op(dve, lambda e: e.tensor_tensor(jf[0:nt, :], xnf[0:nt, :], wr_sb[0:nt, e_ * D:(e_ + 1) * D], ALU.mult),
                           reads=["xnf", "wr_sb"], writes=["jf"])
                        op(dve, lambda e: e.reduce_sum(lg[0:nt, e_:e_ + 1], jf[0:nt, :], AX.X), reads=["jf"], writes=["lg"])
                    op(dve, lambda e: e.tensor_tensor(lg[0:nt, :], lg[0:nt, :], br_sb[0:nt, :], ALU.add), reads=["lg", "br_sb"], writes=["lg"])
                    op(dve, lambda e: e.reduce_max(rt[0:nt, 0:1], lg[0:nt, :], AX.X), reads=["lg"], writes=["rt"])
                    op(dve, lambda e: e.tensor_scalar(rt[0:nt, 8:16], lg[0:nt, :], rt[0:nt, 0:1], -1e30, ALU.is_equal, ALU.mult), reads=["lg", "rt"], writes=["rt"])
                    op(dve, lambda e: e.tensor_tensor(rt[0:nt, 8:16], rt[0:nt, 8:16], lg[0:nt, :], ALU.add), reads=["lg", "rt"], writes=["rt"])
                    op(dve, lambda e: e.reduce_max(rt[0:nt, 1:2], rt[0:nt, 8:16], AX.X), reads=["rt"], writes=["rt"])
                    op(dve, lambda e: e.tensor_scalar(rt[0:nt, 16:24], lg[0:nt, :], rt[0:nt, 1:2], None, ALU.is_ge), reads=["lg", "rt"], writes=["rt"])
                    op(dve, lambda e: e.tensor_scalar(rt[0:nt, 2:3], rt[0:nt, 0:1], -1.0, None, ALU.mult), reads=["rt"], writes=["rt"])
                    op(act, lambda e: e.activation(rt[0:nt, 24:32], lg[0:nt, :], AF.Exp, bias=rt[0:nt, 2:3]), reads=["lg", "rt"], writes=["rt"])
                    op(dve, lambda e: e.tensor_tensor(rt[0:nt, 24:32], rt[0:nt, 24:32], rt[0:nt, 16:24], ALU.mult), reads=["rt"], writes=["rt"])
                    op(dve, lambda e: e.reduce_sum(rt[0:nt, 3:4], rt[0:nt, 24:32], AX.X), reads=["rt"], writes=["rt"])
                    op(dve, lambda e: e.reciprocal(rt[0:nt, 4:5], rt[0:nt, 3:4]), reads=["rt"], writes=["rt"])
                    op(dve, lambda e: e.tensor_scalar(gates_sb[0:nt, ti, :], rt[0:nt, 24:32], rt[0:nt, 4:5], None, ALU.mult), reads=["rt"], writes=["gates"])

        def outproj(es, aT, akey, KC, W_ap, res_in, res_out, cbw, gates_sb=None, gcol=None, tag="o", tiles=None):
            ncb = D // cbw
            wb = [sbt(es, f"{tag}_wb{i}", [128, KC, cbw], BF16) for i in range(2)]
            rin = [sbt(es, f"{tag}_rin{i}", [128, cbw], F32) for i in range(3)]
            Wv = W_ap.rearrange("(c p) m -> p c m", p=128)
            k = 0
            for cb in range(ncb):
                w_ = wb[cb % 2]
                wk_ = f"{tag}_wb{cb % 2}"
                dma(pool, w_[:], Wv[:, :, cb * cbw:(cb + 1) * cbw], writes=[wk_])
                for (ti, t0, nt) in (tiles or TILES):
                    pp = pA[k % 2]
                    pk = f"pA{k % 2}"
                    r_ = rin[k % 3]
                    rk = f"{tag}_rin{k % 3}"
                    k += 1
                    dma(sp, r_[0:nt, :], res_in[t0:t0 + nt, cb * cbw:(cb + 1) * cbw], reads=[("hres", ti, cb)] if (res_in is h or res_in is hm) else [], writes=[rk])
                    for kc in range(KC):
                        op(pe, lambda e: e.matmul(pp[0:nt, 0:cbw], aT[:, kc, t0:t0 + nt], w_[:, kc, :], start=(kc == 0), stop=(kc == KC - 1)),
                           reads=[akey, wk_], writes=[pk], inc=(kc == KC - 1))
                    if gates_sb is None:
                        op(dve, lambda e: e.tensor_tensor(r_[0:nt, :], r_[0:nt, :], pp[0:nt, 0:cbw], ALU.add), reads=[pk, rk], writes=[rk])
                    else:
                        op(dve, lambda e: e.scalar_tensor_tensor(r_[0:nt, :], pp[0:nt, 0:cbw], gates_sb[0:nt, ti, gcol:gcol + 1], r_[0:nt, :], ALU.mult, ALU.add),
                           reads=[pk, rk, "gates"], writes=[rk])
                    dma(sp, res_out[t0:t0 + nt, cb * cbw:(cb + 1) * cbw], r_[0:nt, :], reads=[rk], writes=[("hres", ti, cb)])

        def ffn_block(es, xT, xkey, Wg_ap, Wu_ap, Wd_ap, gates_sb=None, gcol=None, tag="f", tiles=None, btiles=None, Tn=T, res=None):
            NMC = DFE // 128
            actT = sbt(es, f"{tag}_actT", [128, NMC, Tn], BF16)
            wgc = [sbt(es, f"{tag}_wg{i}", [128, 16, 128], BF16) for i in range(2)]
            wuc = [sbt(es, f"{tag}_wu{i}", [128, 16, 128], BF16) for i in range(2)]
            sg = [sbt(es, f"{tag}_sg{i}", [128, 512], F32) for i in range(2)]
            Wgv = Wg_ap.rearrange("(c p) m -> p c m", p=128)
            Wuv = Wu_ap.rearrange("(c p) m -> p c m", p=128)
            k = 0
            for mc in range(NMC):
                g_ = wgc[mc % 2]
                u_ = wuc[mc % 2]
                gk = f"{tag}_wg{mc % 2}"
                uk = f"{tag}_wu{mc % 2}"
                dma(pool, g_[:], Wgv[:, :, mc * 128:(mc + 1) * 128], writes=[gk])
                dma(pool, u_[:], Wuv[:, :, mc * 128:(mc + 1) * 128], writes=[uk])
                for (bi, t0, nt) in (btiles or BTILES):
                    pg = pBk[k % 2]
                    pu = pCk[k % 2]
                    pgk = f"pB{k % 2}"
                    puk = f"pC{k % 2}"
                    s_ = sg[k % 2]
                    sk = f"{tag}_sg{k % 2}"
                    k += 1
                    for kc in range(16):
                        op(pe, lambda e: e.matmul(pg[:, 0:nt], g_[:, kc, :], xT[:, kc, t0:t0 + nt], start=(kc == 0), stop=(kc == 15)),
                           reads=[gk, xkey], writes=[pgk], inc=(kc == 15))
                    for kc in range(16):
                        op(pe, lambda e: e.matmul(pu[:, 0:nt], u_[:, kc, :], xT[:, kc, t0:t0 + nt], start=(kc == 0), stop=(kc == 15)),
                           reads=[uk, xkey], writes=[puk], inc=(kc == 15))
                    op(act, lambda e: e.activation(s_[:, 0:nt], pg[:, 0:nt], AF.Silu), reads=[pgk], writes=[sk])
                    op(dve, lambda e: e.tensor_tensor(actT[:, mc, t0:t0 + nt], s_[:, 0:nt], pu[:, 0:nt], ALU.mult), reads=[sk, puk], writes=[f"{tag}_actT"])
            outproj(es, actT, f"{tag}_actT", NMC, Wd_ap, res if res is not None else h, res if res is not None else h, 256, gates_sb=gates_sb, gcol=gcol, tag=tag + "d", tiles=tiles)

        with ExitStack() as esA:
            if not only_attn:
              xn0T = sbt(esA, "xn0T", [128, 16, T], BF16)
              with ExitStack() as es:
                norm_stage(es, xs, [(g_mix0, xn0T, "xn0T")], tag="n0")
                Tk.barrier()
            if stop_after >= 2 and not only_attn:
              with ExitStack() as es:
                Wh = sbt(es, "Wh", [128, 16, 1536], BF16)
                Wig = sbt(es, "Wig", [128, 16, 128], BF16)
                Wlf = sbt(es, "Wlf", [128, 16, 128], BF16)
                R = [sbt(es, f"R{i}", [128, T], F32) for i in range(4)]
                Z = sbt(es, "Zrow", [128, T], F32)
                bcol = sbt(es, "bcol", [128, 4], F32)
                mfin = sbt(es, "mfin", [128, 4], F32)
                gon = sbt(es, "gon", [128, D], F32)
                Cf = sbt(es, "Cf", [128, 2, 512], F32)
                Cb = sbt(es, "Cb", [128, 2, 512], BF16)
                nf = sbt(es, "nf", [128, 2], F32)
                nb = sbt(es, "nb", [128, 2], BF16)
                w_inv = w_in.rearrange("(c p) m -> p c m", p=128)
                dma(sp, gon[:], g_on, writes=["gon"])
                qk = [sbt(es, f"qk{i}", [128, 512], BF16) for i in range(2)]
                qw = [sbt(es, f"qw{i}", [128, 256], BF16) for i in range(2)]
                vs_ = [sbt(es, f"vs{i}", [128, 512], BF16) for i in range(2)]
                so = [sbt(es, f"so{i}", [128, 512], F32) for i in range(2)]
                cols = [sbt(es, f"cols{i}", [128, 8], F32) for i in range(2)]
                qkT = [sbt(es, f"qkT{i}", [128, 6, 128], BF16) for i in range(2)]
                tmpf2 = [sbt(es, f"tmpf{i}", [128, 128], F32) for i in range(2)]
                WT2 = [sbt(es, f"WT{i}", [128, 128], F32) for i in range(2)]
                STb2 = [sbt(es, f"STb{i}", [128, 128], BF16) for i in range(2)]
                kw2 = [sbt(es, f"kw{i}", [128, 256], BF16) for i in range(2)]
                hf2 = [sbt(es, f"hf{i}", [128, 512], F32) for i in range(2)]
                hj2 = [sbt(es, f"hj{i}", [128, 512], BF16) for i in range(2)]
                hg2 = [sbt(es, f"hg{i}", [128, 512], BF16) for i in range(2)]
                hst2 = [sbt(es, f"hst{i}", [128, 8], F32) for i in range(2)]
                hTs = [sbt(es, f"hTs{i}", [128, 4, 128], BF16) for i in range(2)]
                hTv = hT.rearrange("(c p) t -> p c t", p=128)
                itn = 0
                for hgrp in ([0, 1, 2], [3]):
                    op(pool, lambda e: e.memset(Wig[:], 0.0), writes=["Wig"])
                    op(pool, lambda e: e.memset(Wlf[:], 0.0), writes=["Wlf"])
                    op(dve, lambda e: e.memset(bcol[:], 0.0), writes=["bcol"])
                    op(dve, lambda e: e.memset(Z[:], 0.0), writes=["Z"])
                    for i in range(4):
                        op(dve, lambda e: e.memset(R[i][:], 0.0), writes=[f"R{i}"])
                    gbase = 2 * NH * DK + 2 * NH * DV
                    for hh in hgrp:
                        dma(pool, Wig[:, :, 32 * hgrp.index(hh):32 * hgrp.index(hh) + 1], w_inv[:, :, gbase + hh:gbase + hh + 1], writes=["Wig"], slow=True)
                        dma(pool, Wlf[:, :, 32 * hgrp.index(hh):32 * hgrp.index(hh) + 1], w_inv[:, :, gbase + NH + hh:gbase + NH + hh + 1], writes=["Wlf"], slow=True)
                        dma(sp, bcol[32 * hgrp.index(hh):32 * hgrp.index(hh) + 1, 0:1], bgate[0:1, hh:hh + 1], writes=["bcol"])
                        dma(sp, bcol[32 * hgrp.index(hh):32 * hgrp.index(hh) + 1, 1:2], bgate[0:1, NH + hh:NH + hh + 1], writes=["bcol"])
                        dma(sp, bcol[32 * hgrp.index(hh):32 * hgrp.index(hh) + 1, 2:3], m0[0:1, hh:hh + 1], writes=["bcol"])
                    op(dve, lambda e: e.tensor_scalar(bcol[:, 0:2], bcol[:, 0:2], 1.0 / 15.0, None, ALU.mult), reads=["bcol"], writes=["bcol"])
                    for (bi, t0, nt) in BTILES:
                        for (Wt, wk_, pp, pk, col, Rd, rk) in ((Wig, "Wig", pA[0], "pA0", 0, R[0], "R0"), (Wlf, "Wlf", pA[1], "pA1", 1, R[1], "R1")):
                            for kc in range(16):
                                op(pe, lambda e: e.matmul(pp[:, 0:nt], Wt[:, kc, :], xn0T[:, kc, t0:t0 + nt], start=(kc == 0), stop=(kc == 15)),
                                   reads=[wk_, "xn0T"], writes=[pk], inc=(kc == 15))
                            op(act, lambda e: e.activation(Rd[:, t0:t0 + nt], pp[:, 0:nt], AF.Tanh, bias=bcol[:, col:col + 1], scale=1.0 / 15.0),
                               reads=[pk, "bcol"], writes=[rk])
                    op(dve, lambda e: e.tensor_scalar(R[0][:], R[0][:], 15.0, None, ALU.mult), reads=["R0"], writes=["R0"])
                    op(act, lambda e: e.activation(R[1][:], R[1][:], AF.Exp, scale=-15.0), reads=["R1"], writes=["R1"])
                    op(act, lambda e: e.activation(R[1][:], R[1][:], AF.Ln, bias=1.0), reads=["R1"], writes=["R1"])
                    op(dve, lambda e: e.tensor_scalar(R[1][:], R[1][:], -1.0, None, ALU.mult), reads=["R1"], writes=["R1"])
                    op(dve, lambda e: e.tensor_tensor_scan(R[2][:, 0:S], R[1][:, 0:S], Z[:, 0:S], 0.0, ALU.add, ALU.add), reads=["R1", "Z"], writes=["R2"])
                    op(dve, lambda e: e.tensor_copy(R[2][:, S:T], R[1][:, S:T]), reads=["R1"], writes=["R2"])
                    op(dve, lambda e: e.tensor_tensor_scan(R[3][:, 0:S], R[1][:, 0:S], R[0][:, 0:S], 0.0, ALU.add, ALU.max), reads=["R1", "R0"], writes=["R3"])
                    op(dve, lambda e: e.scalar_tensor_tensor(R[3][:, S:T], R[1][:, S:T], bcol[:, 2:3], R[0][:, S:T], ALU.add, ALU.max), reads=["R1", "R0", "bcol"], writes=["R3"])
                    op(dve, lambda e: e.tensor_copy(mfin[:, 0:1], R[3][:, S - 1:S]), reads=["R3"], writes=["mfin"])
                    op(dve, lambda e: e.tensor_copy(mfin[:, 1:2], R[3][:, S:T]), reads=["R3"], writes=["mfin"])
                    op(dve, lambda e: e.tensor_tensor(R[1][:], R[2][:], R[3][:], ALU.subtract), reads=["R2", "R3"], writes=["R1"])
                    op(dve, lambda e: e.tensor_tensor(R[2][:], R[0][:], R[2][:], ALU.subtract), reads=["R0", "R2"], writes=["R2"])
                    op(act, lambda e: e.activation(R[3][:], R[3][:], AF.Exp, scale=-1.0), reads=["R3"], writes=["R3"])
                    op(dve, lambda e: e.tensor_copy(R[0][:, 0:128], R[1][:, 0:128]), reads=["R1"], writes=["R0"])
                    for i in range(1, 16):
                        op(dve, lambda e: e.tensor_scalar(R[0][:, i * 128:(i + 1) * 128], R[1][:, i * 128:(i + 1) * 128], R[1][:, i * 128 - 1:i * 128], None, ALU.subtract),
                           reads=["R1"], writes=["R0"])
                    op(dve, lambda e: e.tensor_scalar(R[0][:, S:T], R[1][:, S:T], bcol[:, 2:3], None, ALU.add), reads=["R1", "bcol"], writes=["R0"])
                    op(act, lambda e: e.activation(R[0][:], R[0][:], AF.Exp), reads=["R0"], writes=["R0"])
                    Rwi, Ru, Rc, Rem = R[0], R[1], R[2], R[3]
                    for hh in hgrp:
                        dma(sp, pm[0:1, hh:hh + 1], mfin[32 * hgrp.index(hh):32 * hgrp.index(hh) + 1, 0:1], reads=["mfin"], writes=["o_pm"])
                        dma(sp, sm[0:1, hh:hh + 1], mfin[32 * hgrp.index(hh):32 * hgrp.index(hh) + 1, 1:2], reads=["mfin"], writes=["o_sm"])

                    for hh in hgrp:
                        r0 = 32 * hgrp.index(hh)
                        segs = ((hh * DK, 0, DK), (NH * DK + hh * DK, DK, DK), (2 * NH * DK + hh * DV, 2 * DK, DV), (2 * NH * DK + NH * DV + hh * DV, 2 * DK + DV, DV))
                        for (src0, dst0, wdt) in segs:
                            dma(pool, Wh[:, :, dst0:dst0 + wdt], w_inv[:, :, src0:src0 + wdt], writes=["Wh"])
                        op(dve, lambda e: e.memset(Cf[:], 0.0), writes=["Cf"])
                        op(pool, lambda e: e.memset(Cb[:], 0.0), writes=["Cb"])
                        op(dve, lambda e: e.memset(nf[:], 0.0), writes=["nf"])
                        op(pool, lambda e: e.memset(nb[:], 0.0), writes=["nb"])
                        for (ti, t0, nt) in TILES:
                            if ti == 16:
                                dma(sp, pC[hh].rearrange("(c p) e -> p c e", p=128), Cf[:], reads=["Cf"], writes=["o_pC"])
                                for dc in range(2):
                                    dma(sp, pn[hh:hh + 1, dc * 128:(dc + 1) * 128].rearrange("o p -> p o"), nf[:, dc:dc + 1], reads=["nf"], writes=["o_pn"], slow=True)
                                dma(sp, Cf[:], C0[hh].rearrange("(c p) e -> p c e", p=128), writes=["Cf"])
                                for dc in range(2):
                                    dma(sp, nf[:, dc:dc + 1], n0[hh:hh + 1, dc * 128:(dc + 1) * 128].rearrange("o p -> p o"), writes=["nf"], slow=True)
                                op(act, lambda e: e.copy(Cb[:], Cf[:]), reads=["Cf"], writes=["Cb"])
                                op(dve, lambda e: e.tensor_copy(nb[:], nf[:]), reads=["nf"], writes=["nb"])
                            sl = itn % 2
                            itn += 1
                            qk_, qw_, v_, so_, cl_, qT_ = qk[sl], qw[sl], vs_[sl], so[sl], cols[sl], qkT[sl]
                            tmpf, WT, STb, kw, hf, hj, hg, hst = tmpf2[sl], WT2[sl], STb2[sl], kw2[sl], hf2[sl], hj2[sl], hg2[sl], hst2[sl]
                            K_tmpf, K_WT, K_STb, K_kw, K_hf, K_hj, K_hg, K_hst = f"tmpf{sl}", f"WT{sl}", f"STb{sl}", f"kw{sl}", f"hf{sl}", f"hj{sl}", f"hg{sl}", f"hst{sl}"
                            kq, kqw, kv_, kso, kcl, kqT = f"qk{sl}", f"qw{sl}", f"vs{sl}", f"so{sl}", f"cols{sl}", f"qkT{sl}"
                            pq, pv, po_ = pA[sl], pBk[sl], pCk[sl]
                            kpq, kpv, kpo = f"pA{sl}", f"pB{sl}", f"pC{sl}"
                            for (pp, pk, c0) in ((pq, kpq, 0), (pv, kpv, 512), (po_, kpo, 1024)):
                                for kc in range(16):
                                    op(pe, lambda e: e.matmul(pp[0:nt, :], xn0T[:, kc, t0:t0 + nt], Wh[:, kc, c0:c0 + 512], start=(kc == 0), stop=(kc == 15)),
                                       reads=["xn0T", "Wh"], writes=[pk], inc=(kc == 15))
                            pD = pTk
                            psm = pCk[1 - sl] if False else None
                            px = pCk[1 - sl]
                            kpx = f"pC{1 - sl}"
                            for j, Rr in enumerate((Rc, Rwi, Rem)):
                                rkey = ("R2", "R0", "R3")[j]
                                op(pe, lambda e: e.matmul(px[0:nt, j:j + 1], Rr[r0:r0 + 1, t0:t0 + nt], onf[r0:r0 + 1, 0:1], start=True, stop=True),
                                   reads=[rkey, "cf"], writes=[kpx], inc=False)
                            op(pe, lambda e: e.matmul(px[:, 4:5], onf[r0:r0 + 1, 0:128], Rwi[r0:r0 + 1, t0 + nt - 1:t0 + nt], start=True, stop=True),
                               reads=["R0", "cf"], writes=[kpx], inc=False)
                            op(pe, lambda e: e.matmul(px[0:nt, 128:128 + nt], onf[r0:r0 + 1, 0:nt], Ru[r0:r0 + 1, t0:t0 + nt], start=True, stop=True),
                               reads=["R1", "cf"], writes=[kpx])
                            op(act, lambda e: e.copy(cl_[0:nt, 0:3], px[0:nt, 0:3]), reads=[kpx], writes=[kcl])
                            op(act, lambda e: e.copy(cl_[:, 4:5], px[:, 4:5]), reads=[kpx], writes=[kcl])
                            op(dve, lambda e: e.tensor_tensor(tmpf[0:nt, 0:nt], px[0:nt, 128:128 + nt], mcur_f[0:nt, 0:nt], ALU.add), reads=[kpx, "cf"], writes=[K_tmpf])
                            op(act, lambda e: e.activation(WT[0:nt, 0:nt], tmpf[0:nt, 0:nt], AF.Exp, bias=cl_[0:nt, 0:1]), reads=[K_tmpf, kcl], writes=[K_WT])
                            op(act, lambda e: e.copy(qk_[0:nt, 0:256], pq[0:nt, 0:256]), reads=[kpq], writes=[kq])
                            op(dve, lambda e: e.tensor_scalar(qk_[0:nt, 256:512], pq[0:nt, 256:512], DK ** -0.5, None, ALU.mult), reads=[kpq], writes=[kq])
                            op(dve, lambda e: e.tensor_scalar(qw_[0:nt, :], pq[0:nt, 0:256], cl_[0:nt, 1:2], None, ALU.mult), reads=[kpq, kcl], writes=[kqw])
                            op(act, lambda e: e.copy(v_[0:nt, :], pv[0:nt, :]), reads=[kpv], writes=[kv_])
                            op(act, lambda e: e.activation(so_[0:nt, :], po_[0:nt, :], AF.Sigmoid), reads=[kpo], writes=[kso])
                            pt = pTk[sl]
                            kpt = f"pT{sl}"
                            srcs = ((qk_, kq, 0), (qk_, kq, 128), (qw_, kqw, 0), (qw_, kqw, 128), (qk_, kq, 256), (qk_, kq, 384))
                            for j, (sv, skk, c0) in enumerate(srcs):
                                op(pe, lambda e: e.transpose(pt[:, j, 0:nt], sv[0:nt, c0:c0 + 128], idb[0:nt, 0:nt]), reads=[skk, "idb"], writes=[kpt], inc=(j == 5))
                            op(dve, lambda e: e.tensor_copy(qT_[:, :, 0:nt], pt[:, 0:6, 0:nt]), reads=[kpt], writes=[kqT])
                            for dc in range(2):
                                op(pe, lambda e: e.matmul(px[0:nt, 256:256 + nt], qT_[:, 4 + dc, 0:nt], qT_[:, dc, 0:nt], start=(dc == 0), stop=(dc == 1)),
                                   reads=[kqT], writes=[kpx], inc=(dc == 1))
                            op(dve, lambda e: e.tensor_tensor(STb[0:nt, 0:nt], px[0:nt, 256:256 + nt], WT[0:nt, 0:nt], ALU.mult), reads=[kpx, K_WT], writes=[K_STb])
                            op(dve, lambda e: e.tensor_scalar(kw[0:nt, :], qk_[0:nt, 256:512], WT[0:nt, nt - 1:nt], None, ALU.mult), reads=[kq, K_WT], writes=[K_kw])
                            pnum = pq
                            op(pe, lambda e: e.matmul(pnum[0:nt, :], STb[0:nt, 0:nt], v_[0:nt, :], start=True, stop=False), reads=[K_STb, kv_, kq, kqw], writes=[kpq], inc=False)
                            for dc in range(2):
                                op(pe, lambda e: e.matmul(pnum[0:nt, :], qT_[:, 2 + dc, 0:nt], Cb[:, dc, :], start=False, stop=(dc == 1)), reads=[kqT, "Cb"], writes=[kpq], inc=(dc == 1))
                            op(pe, lambda e: e.matmul(px[0:nt, 8:9], STb[0:nt, 0:nt], onb[0:nt, 0:1], start=True, stop=False), reads=[K_STb, "onb"], writes=[kpx], inc=False)
                            for dc in range(2):
                                op(pe, lambda e: e.matmul(px[0:nt, 8:9], qT_[:, 2 + dc, 0:nt], nb[:, dc:dc + 1], start=False, stop=(dc == 1)), reads=[kqT, "nb"], writes=[kpx], inc=(dc == 1))
                            op(act, lambda e: e.activation(hst[0:nt, 6:7], px[0:nt, 8:9], AF.Abs), reads=[kpx], writes=[K_hst])
                            op(dve, lambda e: e.tensor_tensor(hst[0:nt, 0:1], hst[0:nt, 6:7], cl_[0:nt, 2:3], ALU.max), reads=[K_hst, kcl], writes=[K_hst])
                            op(dve, lambda e: e.reciprocal(hst[0:nt, 1:2], hst[0:nt, 0:1]), reads=[K_hst], writes=[K_hst])
                            op(dve, lambda e: e.tensor_scalar(hf[0:nt, :], pnum[0:nt, :], hst[0:nt, 1:2], None, ALU.mult), reads=[kpq, K_hst], writes=[K_hf])
                            for dc in range(2):
                                pcs = (pv, po_)[dc]
                                kpcs = (kpv, kpo)[dc]
                                op(pe, lambda e: e.matmul(pcs[:, :], kw[0:nt, dc * 128:(dc + 1) * 128], v_[0:nt, :], start=True, stop=True), reads=[K_kw, kv_, kso], writes=[kpcs])
                                op(pe, lambda e: e.matmul(px[:, 12 + dc:13 + dc], kw[0:nt, dc * 128:(dc + 1) * 128], onb[0:nt, 0:1], start=True, stop=True), reads=[K_kw, "onb"], writes=[kpx])
                                op(dve, lambda e: e.scalar_tensor_tensor(Cf[:, dc, :], Cf[:, dc, :], cl_[:, 4:5], pcs[:, :], ALU.mult, ALU.add), reads=[kpcs, kcl, "Cf"], writes=["Cf"])
                            op(dve, lambda e: e.scalar_tensor_tensor(nf[:, :], nf[:, :], cl_[:, 4:5], px[:, 12:14], ALU.mult, ALU.add), reads=[kpx, kcl, "nf"], writes=["nf"])
                            op(act, lambda e: e.copy(Cb[:], Cf[:]), reads=["Cf"], writes=["Cb"])
                            op(dve, lambda e: e.tensor_copy(nb[:], nf[:]), reads=["nf"], writes=["nb"])
                            op(act, lambda e: e.activation(hj[0:nt, :], hf[0:nt, :], AF.Square, accum_out=hst[0:nt, 2:3]), reads=[K_hf], writes=[K_hj, K_hst])
                            op(dve, lambda e: e.tensor_scalar(hst[0:nt, 3:4], hst[0:nt, 2:3], 1.0 / DV, EPS, ALU.mult, ALU.add), reads=[K_hst], writes=[K_hst])
                            op(act, lambda e: e.activation(hst[0:nt, 4:5], hst[0:nt, 3:4], AF.Sqrt), reads=[K_hst], writes=[K_hst])
                            op(dve, lambda e: e.reciprocal(hst[0:nt, 5:6], hst[0:nt, 4:5]), reads=[K_hst], writes=[K_hst])
                            op(dve, lambda e: e.scalar_tensor_tensor(hf[0:nt, :], hf[0:nt, :], hst[0:nt, 5:6], gon[0:nt, hh * DV:(hh + 1) * DV], ALU.mult, ALU.mult), reads=[K_hf, K_hst, "gon"], writes=[K_hf])
                            op(dve, lambda e: e.tensor_tensor(hg[0:nt, :], hf[0:nt, :], so_[0:nt, :], ALU.mult), reads=[K_hf, kso], writes=[K_hg])
                            for j in range(4):
                                op(pe, lambda e: e.transpose(pt[:, j, 0:nt], hg[0:nt, j * 128:(j + 1) * 128], idb[0:nt, 0:nt]), reads=[K_hg, "idb", kqT], writes=[kpt], inc=(j == 3))
                            ht_ = hTs[sl]
                            kht = f"hTs{sl}"
                            op(act, lambda e: e.copy(ht_[:, :, 0:nt], pt[:, 0:4, 0:nt]), reads=[kpt], writes=[kht])
                            dma(sp, hTv[:, hh * 4:(hh + 1) * 4, t0:t0 + nt], ht_[:, :, 0:nt], reads=[kht], writes=[("hT", hh, ti)], slow=(nt == 1))
                        dma(sp, sC[hh].rearrange("(c p) e -> p c e", p=128), Cf[:], reads=["Cf"], writes=["o_sC"])
                        for dc in range(2):
                            dma(sp, sn[hh:hh + 1, dc * 128:(dc + 1) * 128].rearrange("o p -> p o"), nf[:, dc:dc + 1], reads=["nf"], writes=["o_sn"], slow=True)
                Tk.barrier()

        if stop_after >= 3 and not only_attn:
            with ExitStack() as es:
                aT = sbt(es, "aT", [128, 16, T], BF16)
                dma(sp, aT[:], hT.rearrange("(c p) t -> p c t", p=128), writes=["aT"])
                outproj(es, aT, "aT", 16, w_out, xs, h, 512, tag="wo")
                Tk.barrier()
        if stop_after >= 5 and not only_attn:
            with ExitStack() as esA:
                xn1T = sbt(esA, "xn1T", [128, 16, T], BF16)
                with ExitStack() as es:
                    norm_stage(es, h, [(g_ffn0, xn1T, "xn1T")], tag="n1")
                    Tk.barrier()
                for fb in range(2):
                    with ExitStack() as es:
                        ffn_block(es, xn1T, "xn1T", wg[:, fb * DFE:(fb + 1) * DFE], wu[:, fb * DFE:(fb + 1) * DFE], wd[fb * DFE:(fb + 1) * DFE, :], tag=f"f{fb}")
                        Tk.barrier()
        if stop_after >= 8:
            with ExitStack() as esA:
                xkvT = sbt(esA, "xkvT", [128, 16, T], BF16)
                with ExitStack() as esB:
                    xqT = sbt(esB, "xqT", [128, 16, T], BF16)
                    with ExitStack() as es:
                        norm_stage(es, xs if only_attn else h, [(g_kv, xkvT, "xkvT"), (g_mix1, xqT, "xqT")], tag="n2")
                        Tk.barrier()
                    with ExitStack() as es:
                        wqc = [sbt(es, f"wqc{i}", [128, 16, 128], BF16) for i in range(2)]
                        qTs = [sbt(es, f"qTs{i}", [128, T], BF16) for i in range(2)]
                        w_qv = w_q.rearrange("(c p) m -> p c m", p=128)
                        k = 0
                        for mc in range(48):
                            w_ = wqc[mc % 2]
                            wk_ = f"wqc{mc % 2}"
                            q_ = qTs[mc % 2]
                            qk_ = f"qTs{mc % 2}"
                            dma(pool, w_[:], w_qv[:, :, mc * 128:(mc + 1) * 128], writes=[wk_])
                            for (bi, t0, nt) in BTILES:
                                pp = pA[k % 2]
                                pk = f"pA{k % 2}"
                                k += 1
                                for kc in range(16):
                                    op(pe, lambda e: e.matmul(pp[:, 0:nt], w_[:, kc, :], xqT[:, kc, t0:t0 + nt], start=(kc == 0), stop=(kc == 15)),
                                       reads=[wk_, "xqT"], writes=[pk], inc=(kc == 15))
                                copy_op(evac_eng(), q_[:, t0:t0 + nt], pp[:, 0:nt], [pk], [qk_])
                            dma(sp, qTd[mc * 128:(mc + 1) * 128, :], q_[:], reads=[qk_], writes=[("qTd", mc)])
                        Tk.barrier()
                with ExitStack() as es:
                    Oacc = sbt(es, "Oacc", [128, T], F32)
                    Lacc = sbt(es, "Lacc", [128, T], F32)
                    oTh = sbt(es, "oTh", [128, T], BF16)
                    wkv = [sbt(es, f"wkv{i}", [128, 16, 256], BF16) for i in range(2)]
                    qTh = [sbt(es, f"qTh{i}", [128, T], BF16) for i in range(2)]
                    KT = sbt(es, "KT", [128, 16, 128], BF16)
                    Vall = sbt(es, "Vall", [128, 16, 128], BF16)
                    kvf = [sbt(es, f"kvf{i}", [128, 256], F32) for i in range(3)]
                    kvb = [sbt(es, f"kvb{i}", [128, 128], BF16) for i in range(2)]
                    cKV = sbt(es, "cKV", [128, 2, 128], BF16)
                    cKT = sbt(es, "cKT", [128, 128], BF16)
                    KsT = sbt(es, "KsT", [128, 1], BF16)
                    Vs = sbt(es, "Vs", [1, 128], BF16)
                    Pb = [sbt(es, f"Pb{i}", [128, 2, 128], BF16) for i in range(3)]
                    w_kvv = w_kv.rearrange("(c p) m -> p c m", p=128)
                    scale = 128.0 ** -0.5
                    it = 0
                    fcount = 0
                    Tk.limit = limit
                    for hd in range(nheads):
                        op(pool, lambda e: e.memset(Oacc[:], 0.0), writes=["Oacc"])
                        op(pool, lambda e: e.memset(Lacc[:], 0.0), writes=["Lacc"])
                        for g, (window, r) in enumerate(GROUPS[:ngroups]):
                            nbk = 16 // r
                            sl = it % 2
                            it += 1
                            wk_ = wkv[sl]
                            kwk = f"wkv{sl}"
                            q_ = qTh[sl]
                            kq_ = f"qTh{sl}"
                            ck = ((g * 2 + 0) * 16 + hd) * 128
                            cv = ((g * 2 + 1) * 16 + hd) * 128
                            dma(pool, wk_[:, :, 0:128], w_kvv[:, :, ck:ck + 128], writes=[kwk])
                            dma(pool, wk_[:, :, 128:256], w_kvv[:, :, cv:cv + 128], writes=[kwk])
                            dma(sp, q_[:], qTd[(g * 16 + hd) * 128:(g * 16 + hd + 1) * 128, :], reads=[("qTd", g * 16 + hd)], writes=[kq_])
                            if dbg != 12:
                                dma(pool, cKV[:], caches[g][DS(0, 128, r), :, hd, :], writes=["cKV"])
                            for f in range(17 if dbg != 13 else 16):
                                if f < 16:
                                    res, blk = f // nbk, f % nbk
                                    start = res + r * 128 * blk
                                    nt = 128
                                    tok = DS(start, 128, r)
                                else:
                                    nt = 1
                                    tok = DS(S, 1, 1)
                                pp = pA[fcount % 2]
                                pk = f"pA{fcount % 2}"
                                kf_ = kvf[fcount % 3]
                                kkf = f"kvf{fcount % 3}"
                                kb_ = kvb[fcount % 2]
                                kkb = f"kvb{fcount % 2}"
                                pt = pTk[fcount % 2]
                                kpt = f"pT{fcount % 2}"
                                fcount += 1
                                for kc in range(16):
                                    op(pe, lambda e: e.matmul(pp[0:nt, 0:256], xkvT[:, kc, tok], wk_[:, kc, :], start=(kc == 0), stop=(kc == 15)),
                                       reads=["xkvT", kwk], writes=[pk], inc=(kc == 15))
                                op(act, lambda e: e.copy(kf_[0:nt, :], pp[0:nt, 0:256]), reads=[pk], writes=[kkf])
                                op(dve, lambda e: e.tensor_copy(kb_[0:nt, :], pp[0:nt, 0:128]), reads=[pk], writes=[kkb])
                                if f < 16:
                                    op(dve, lambda e: e.tensor_copy(Vall[:, f, :], pp[:, 128:256]), reads=[pk], writes=[("Vall", f)])
                                    op(pe, lambda e: e.transpose(pt[:, 0, :], kb_[:, :], idb[:]), reads=[kkb, "idb"], writes=[kpt])
                                    op(act, lambda e: e.copy(KT[:, f, :], pt[:, 0, :]), reads=[kpt], writes=[("KT", f)])
                                    kview = kf_[:].rearrange("p (a d) -> p a d", a=2)
                                    if dbg == 11:
                                        pass
                                    elif g == 0 and f == 15:
                                        dma(sp, pkv[0][:, :, hd, :], kview, reads=[kkf], writes=["o_pkv"])
                                    elif g == 1 and blk == 3:
                                        dma(sp, pkv[1][DS(res, 128, 4), :, hd, :], kview, reads=[kkf], writes=["o_pkv"])
                                    elif g == 2:
                                        dma(sp, pkv[2][DS(res, 128, 16), :, hd, :], kview, reads=[kkf], writes=["o_pkv"])
                                else:
                                    op(dve, lambda e: e.tensor_copy(Vs[0:1, :], pp[0:1, 128:256]), reads=[pk], writes=["Vs"])
                                    op(pe, lambda e: e.transpose(pt[:, 0, 0:1], kb_[0:1, :], idb[0:1, 0:1]), reads=[kkb, "idb"], writes=[kpt])
                                    op(act, lambda e: e.copy(KsT[:, 0:1], pt[:, 0, 0:1]), reads=[kpt], writes=["KsT"])
                                    if dbg != 11:
                                        dma(sp, skv[g, :, hd, :], kf_[0:1, :].rearrange("p (a d) -> p a d", a=2), reads=[kkf], writes=["o_skv"])
                            if dbg in (1, 11, 12, 13, 14):
                                continue
                            ptc = pTk[fcount % 2]
                            kptc = f"pT{fcount % 2}"
                            op(pe, lambda e: e.transpose(ptc[:, 1, :], cKV[:, 0, :], idb[:]), reads=["cKV", "idb"], writes=[kptc])
                            op(act, lambda e: e.copy(cKT[:], ptc[:, 1, :]), reads=[kptc], writes=["cKT"])
                            for f in range(16 if dbg != 3 else 0):
                                res, blk = f // nbk, f % nbk
                                start = res + r * 128 * blk
                                qv = q_[:, DS(start, 128, r)]
                                keys = ([(f - 1, mprev_b, "mprev_b")] if blk > 0 else []) + [(f, mcur_b, "mcur_b")]
                                ps_ = (pBk[0], pBk[1], pA[0])[f % 3]
                                kps = ("pB0", "pB1", "pA0")[f % 3]
                                po2 = (pCk[0], pCk[1], pA[1])[f % 3]
                                kpo2 = ("pC0", "pC1", "pA1")[f % 3]
                                P_ = Pb[f % 3]
                                kP = f"Pb{f % 3}"
                                nk = len(keys)
                                for s_i, (kfi, mk, mkk) in enumerate(keys):
                                    op(pe, lambda e: e.matmul(ps_[:, s_i * 128:(s_i + 1) * 128], KT[:, kfi, :], qv, start=True, stop=False),
                                       reads=[("KT", kfi), kq_], writes=[kps], inc=False)
                                    op(pe, lambda e: e.matmul(ps_[:, s_i * 128:(s_i + 1) * 128], idb[:], mk[:], start=False, stop=True),
                                       reads=["idb", mkk], writes=[kps], inc=(s_i == nk - 1))
                                op(act, lambda e: e.activation(P_[:, 0:nk, :], ps_[:, 0:nk * 128].rearrange("p (a q) -> p a q", a=nk), AF.Exp, scale=scale),
                                   reads=[kps], writes=[kP])
                                for s_i, (kfi, mk, mkk) in enumerate(keys):
                                    op(pe, lambda e: e.matmul(po2[:, 0:128], Vall[:, kfi, :], P_[:, s_i, :], start=(s_i == 0), stop=(s_i == nk - 1)),
                                       reads=[("Vall", kfi), kP], writes=[kpo2], inc=False)
                                for s_i, (kfi, mk, mkk) in enumerate(keys):
                                    op(pe, lambda e: e.matmul(po2[:, 128:256], onb[:], P_[:, s_i, :], start=(s_i == 0), stop=(s_i == nk - 1)),
                                       reads=["onb", kP], writes=[kpo2], inc=(s_i == nk - 1))
                                op(dve, lambda e: e.tensor_tensor(Oacc[:, DS(start, 128, r)], Oacc[:, DS(start, 128, r)], po2[:, 0:128], ALU.add), reads=[kpo2, "Oacc"], writes=["Oacc"])
                                op(dve, lambda e: e.tensor_tensor(Lacc[:, DS(start, 128, r)], Lacc[:, DS(start, 128, r)], po2[:, 128:256], ALU.add), reads=[kpo2, "Lacc"], writes=["Lacc"])
                            if dbg == 2:
                                continue
                            ps_ = pBk[0]
                            po2 = pCk[0]
                            P_ = Pb[0]
                            qs_ = q_[:, S:T]
                            op(pe, lambda e: e.matmul(ps_[:, 0:1], cKT[:], qs_, start=True, stop=True), reads=["cKT", kq_], writes=["pB0"])
                            op(pe, lambda e: e.matmul(ps_[0:1, 128:129], KsT[:, 0:1], qs_, start=True, stop=True), reads=["KsT", kq_], writes=["pB0"])
                            op(act, lambda e: e.activation(P_[:, 0, 0:1], ps_[:, 0:1], AF.Exp, scale=scale), reads=["pB0"], writes=["Pb0"])
                            op(act, lambda e: e.activation(P_[0:1, 1, 0:1], ps_[0:1, 128:129], AF.Exp, scale=scale), reads=["pB0"], writes=["Pb0"])
                            op(pe, lambda e: e.matmul(po2[:, 0:1], cKV[:, 1, :], P_[:, 0, 0:1], start=True, stop=False), reads=["cKV", "Pb0"], writes=["pC0"], inc=False)
                            op(pe, lambda e: e.matmul(po2[:, 0:1], Vs[0:1, :], P_[0:1, 1, 0:1], start=False, stop=True), reads=["Vs", "Pb0"], writes=["pC0"], inc=False)
                            op(pe, lambda e: e.matmul(po2[:, 128:129], onb[:], P_[:, 0, 0:1], start=True, stop=False), reads=["onb", "Pb0"], writes=["pC0"], inc=False)
                            op(pe, lambda e: e.matmul(po2[:, 128:129], onb[0:1, :], P_[0:1, 1, 0:1], start=False, stop=True), reads=["onb", "Pb0"], writes=["pC0"])
                            op(dve, lambda e: e.tensor_tensor(Oacc[:, S:T], Oacc[:, S:T], po2[:, 0:1], ALU.add), reads=["pC0", "Oacc"], writes=["Oacc"])
                            op(dve, lambda e: e.tensor_tensor(Lacc[:, S:T], Lacc[:, S:T], po2[:, 128:129], ALU.add), reads=["pC0", "Lacc"], writes=["Lacc"])
                        op(dve, lambda e: e.reciprocal(Lacc[:], Lacc[:]), reads=["Lacc"], writes=["Lacc"])
                        op(dve, lambda e: e.tensor_tensor(oTh[:], Oacc[:], Lacc[:], ALU.mult), reads=["Oacc", "Lacc"], writes=["oTh"])
                        dma(sp, oT[hd * 128:(hd + 1) * 128, :], oTh[:], reads=["oTh"], writes=[("oT", hd)])
                    Tk.limit = None
                    Tk.dead = False
                    Tk.barrier()
        if stop_after >= 9:
            with ExitStack() as es:
                aT = sbt(es, "aT2", [128, 16, T], BF16)
                dma(sp, aT[:], oT.rearrange("(c p) t -> p c t", p=128), writes=["aT2"])
                outproj(es, aT, "aT2", 16, w_ao, h, h, 512, tag="ao")
                Tk.barrier()
        if stop_after >= 11:
            with ExitStack() as es:
                selt = sbt(es, "selt", [128, 2], F32)
                lo = [sbt(es, f"blo{i}", [128, D], F32) for i in range(2)]
                hi = [sbt(es, f"bhi{i}", [128, D], F32) for i in range(2)]
                dma(sp, selt[:], selv, writes=["selt"])
                for j in range(8):
                    l_, h_ = lo[j % 2], hi[j % 2]
                    lk, hk = f"blo{j % 2}", f"bhi{j % 2}"
                    dma(sp, l_[:], h[j * 128:(j + 1) * 128, :], writes=[lk])
                    dma(sp, h_[:], h[1024 + j * 128:1024 + (j + 1) * 128, :], writes=[hk])
                    op(dve, lambda e: e.tensor_scalar(l_[:], l_[:], selt[:, 0:1], None, ALU.mult), reads=[lk, "selt"], writes=[lk])
                    op(dve, lambda e: e.scalar_tensor_tensor(l_[:], h_[:], selt[:, 1:2], l_[:], ALU.mult, ALU.add), reads=[hk, lk, "selt"], writes=[lk])
                    dma(sp, hm[j * 128:(j + 1) * 128, :], l_[:], reads=[lk], writes=[("hm", j)])
                dma(sp, lo[0][0:1, :], h[S:T, :], writes=["blo0"])
                dma(sp, hm[1024:1025, :], lo[0][0:1, :], reads=["blo0"], writes=[("hm", 8)])
                Tk.barrier()
            with ExitStack() as esA:
                xn3T = sbt(esA, "xn3T", [128, 16, TM], BF16)
                gates_sb = sbt(esA, "gates", [128, 9, NE], F32)
                with ExitStack() as es:
                    norm_stage(es, hm, [(g_ffn1, xn3T, "xn3T")], router=gates_sb, tag="n3", tiles=MTILES)
                    Tk.barrier()
                for e_ in range(NE):
                    with ExitStack() as es:
                        ffn_block(es, xn3T, "xn3T", mwg[e_], mwu[e_], mwd[e_], gates_sb=gates_sb, gcol=e_, tag=f"m{e_}",
                                  tiles=MTILES, btiles=MBTILES, Tn=TM, res=hm)
                        Tk.barrier()
        with ExitStack() as es:
            xin = [sbt(es, f"fx{i}", [128, D], F32) for i in range(2)]
            yo = [sbt(es, f"fy{i}", [128, D], F32) for i in range(2)]
            junk = sbt(es, "fjunk", [128, D], BF16)
            st = sbt(es, "fst", [128, 4], F32)
            gt = sbt(es, "fg", [128, D], F32)
            dma(sp, gt[:], g_fin, writes=["fg"])
            src = hm
            for (ti, t0, nt) in MTILES:
                x_ = xin[ti % 2]
                xk = f"fx{ti % 2}"
                y_ = yo[ti % 2]
                yk = f"fy{ti % 2}"
                dma(sp, x_[0:nt, :], src[t0:t0 + nt, :], writes=[xk])
                op(act, lambda e: e.activation(junk[0:nt, :], x_[0:nt, :], AF.Square, accum_out=st[0:nt, 0:1]), reads=[xk], writes=["fjunk", "fst"])
                op(dve, lambda e: e.tensor_scalar(st[0:nt, 1:2], st[0:nt, 0:1], 1.0 / D, EPS, ALU.mult, ALU.add), reads=["fst"], writes=["fst"])
                op(act, lambda e: e.activation(st[0:nt, 2:3], st[0:nt, 1:2], AF.Sqrt), reads=["fst"], writes=["fst"])
                op(dve, lambda e: e.reciprocal(st[0:nt, 3:4], st[0:nt, 2:3]), reads=["fst"], writes=["fst"])
                op(dve, lambda e: e.scalar_tensor_tensor(y_[0:nt, :], x_[0:nt, :], st[0:nt, 3:4], gt[0:nt, :], ALU.mult, ALU.mult), reads=[xk, "fst", "fg"], writes=[yk])
                dma(sp, y[t0:t0 + nt, :], y_[0:nt, :], reads=[yk], writes=["o_y"])
            Tk.barrier()
    return nc


def _consts():
    c = np.zeros((128, 768), np.float32)
    c[:, 0:128] = np.eye(128, dtype=np.float32)
    c[:, 128:256] = 1.0
    k = np.arange(128)[:, None]
    q = np.arange(128)[None, :]
    c[:, 256:384] = np.where(k <= q, 0.0, NEG)
    c[:, 384:512] = np.where(k >= q, 0.0, NEG)
    c[:, 512:640] = np.where(k <= q, 0.0, -1e30)
    return c


_NC_CACHE = {}


def kernel(x_prompt, x_sample, state_mlstm_C, state_mlstm_n, state_mlstm_m, cache_kv_w128, cache_kv_w512,
           cache_kv_w2048, norm_mix, norm_ffn, mlstm_w_in, mlstm_b_gates, mlstm_out_norm, mlstm_w_out, kv_norm,
           w_kv, attn_w_q, attn_w_out, ffn_w_gate, ffn_w_up, ffn_w_down, moe_w_router, moe_b_router, moe_w_gate,
           moe_w_up, moe_w_down, final_norm, _stop_after=99):
    f = lambda a: np.ascontiguousarray(np.asarray(a, dtype=np.float32))
    rep = lambda v: np.ascontiguousarray(np.broadcast_to(np.asarray(v, np.float32).reshape(1, -1), (128, np.asarray(v).size)))
    if _stop_after not in _NC_CACHE:
        _NC_CACHE[_stop_after] = build_program(_stop_after)
    nc = _NC_CACHE[_stop_after]
    shared = {
        "cst": _consts(),
        "g_mix0": rep(norm_mix[0]), "g_ffn0": rep(norm_ffn[0]), "g_kv": rep(kv_norm), "g_mix1": rep(norm_mix[1]),
        "g_ffn1": rep(norm_ffn[1]), "g_fin": rep(final_norm), "g_on": rep(mlstm_out_norm[0]),
        "w_in": f(mlstm_w_in[0]), "bgate": f(mlstm_b_gates).reshape(1, 8), "w_out": f(mlstm_w_out[0]), "w_kv": f(w_kv),
        "w_q": f(attn_w_q[0]), "w_ao": f(attn_w_out[0]), "wg": f(ffn_w_gate[0]), "wu": f(ffn_w_up[0]), "wd": f(ffn_w_down[0]),
        "wrT": rep(np.asarray(moe_w_router[0], np.float32).T.reshape(-1)), "brr": rep(moe_b_router[0]),
        "mwg": f(moe_w_gate[0]), "mwu": f(moe_w_up[0]), "mwd": f(moe_w_down[0]),
    }
    xp = np.asarray(x_prompt, np.float32)
    xsm = np.asarray(x_sample, np.float32)
    in_maps = []
    for c in range(8):
        m = dict(shared)
        m["xs"] = np.ascontiguousarray(np.concatenate([xp[c % 4], xsm[c]], axis=0))
        m["selv"] = np.ascontiguousarray(np.broadcast_to(np.array([[1.0, 0.0]] if c < 4 else [[0.0, 1.0]], np.float32), (128, 2)))
        m["C0"] = f(state_mlstm_C[0, c])
        m["n0"] = f(state_mlstm_n[0, c])
        m["m0"] = f(state_mlstm_m[0, c]).reshape(1, 4)
        m["ck128"] = f(cache_kv_w128[c])
        m["ck512"] = f(cache_kv_w512[c])
        m["ck2048"] = f(cache_kv_w2048[c])
        in_maps.append(m)
    res = run_bass_kernel_spmd(nc, in_maps, core_ids=list(range(8))).results
    g = lambda c, n: np.asarray(res[c][n], dtype=np.float32)
    y_prompt = np.stack([np.concatenate([g(b, "y")[:1024], g(b + 4, "y")[:1024]], axis=0) for b in range(4)])
    y_sample = np.stack([g(c, "y")[1024:1025] for c in range(8)])
    p_C = np.stack([g(b, "pC") for b in range(4)])[None]
    p_n = np.stack([g(b, "pn") for b in range(4)])[None]
    p_m = np.stack([g(b, "pm")[0] for b in range(4)])[None]
    p_kv = [np.stack([g(b, f"pkv{i}") for b in range(4)]) for i in range(3)]
    s_C = np.stack([g(c, "sC") for c in range(8)])[None]
    s_n = np.stack([g(c, "sn") for c in range(8)])[None]
    s_m = np.stack([g(c, "sm")[0] for c in range(8)])[None]
    s_kv = [np.stack([g(c, "skv")[i][None] for c in range(8)]) for i in range(3)]
    return (y_prompt, y_sample, p_C, p_n, p_m, p_kv[0], p_kv[1], p_kv[2], s_C, s_n, s_m, s_kv[0], s_kv[1], s_kv[2])
```

```python
from contextlib import ExitStack

import numpy as np
import concourse.bass as bass
import concourse.mybir as mybir
from concourse.bass_utils import run_bass_kernel_spmd

F32 = mybir.dt.float32
BF16 = mybir.dt.bfloat16
ALU = mybir.AluOpType
AF = mybir.ActivationFunctionType
AX = mybir.AxisListType
DS = bass.DynSlice

D = 2048
S = 2048
T = 2049
NH = 4
DK = 256
DV = 512
DFF = 5632
NE = 8
DFE = 2816
EPS = 1e-6
GROUPS = ((128, 1), (512, 4), (2048, 16))
NEG = -30000.0

TM = 1025
TILES = [(i, i * 128, 128) for i in range(16)] + [(16, 2048, 1)]
MTILES = [(i, i * 128, 128) for i in range(8)] + [(8, 1024, 1)]
MBTILES = [(i, i * 512, 512) for i in range(2)] + [(2, 1024, 1)]
BTILES = [(i, i * 512, 512) for i in range(4)] + [(4, 2048, 1)]


class Buf:
    __slots__ = ("w", "r")

    def __init__(self):
        self.w = None
        self.r = []


class Q:
    def __init__(self, T_, eng, name, is_pe=False):
        self.eng = eng
        self.name = name
        self.sem = T_.newsem(name)
        self.count = 0
        self.seen = {}
        self.is_pe = is_pe
        self.dsems = []
        self.dvals = []
        self.dnext = 0

    def wait(self, ev):
        if ev is None:
            return
        sem, val = ev
        if self.is_pe and sem is self.sem:
            return
        if self.seen.get(sem, 0) >= val:
            return
        self.eng.wait_ge(sem, val)
        self.seen[sem] = val


class _Stop(Exception):
    pass


class Tracker:
    limit = None
    dead = False

    def _tick(self, inc=True):
        if self.dead:
            return True
        if self.limit is not None and inc:
            if self.limit <= 0:
                self.dead = True
                return True
            self.limit -= 1
        return False

    def __init__(self, nc, es):
        self.nc = nc
        self.es = es
        self.bufs = {}
        self.pe = Q(self, nc.tensor, "pe", is_pe=True)
        self.act = Q(self, nc.scalar, "act")
        self.dve = Q(self, nc.vector, "dve")
        self.pool = Q(self, nc.gpsimd, "pool")
        self.sp = Q(self, nc.sync, "sp")
        self.qs = [self.pe, self.act, self.dve, self.pool, self.sp]
        for q, n in ((self.sp, 12), (self.pool, 8)):
            for i in range(n):
                q.dsems.append(self.newsem(f"{q.name}_d{i}"))
                q.dvals.append(0)

    def newsem(self, name):
        return self.es.enter_context(self.nc.semaphore(f"s_{name}"))

    def B(self, key):
        b = self.bufs.get(key)
        if b is None:
            b = self.bufs[key] = Buf()
        return b

    def _deps(self, q, reads, writes):
        for k in reads:
            q.wait(self.B(k).w)
        for k in writes:
            b = self.B(k)
            q.wait(b.w)
            for ev in b.r:
                q.wait(ev)

    def _commit(self, ev, reads, writes):
        for k in reads:
            b = self.B(k)
            b.r.append(ev)
            if len(b.r) > 32:
                b.r = b.r[-32:]
        for k in writes:
            b = self.B(k)
            b.w = ev
            b.r = []

    def op(self, q, fn, reads=(), writes=(), inc=True):
        if self._tick(inc):
            return None
        pr = [k for k in reads if isinstance(k, str) and k[0] == "p" and k[1] in "ABCT" and len(k) == 3]
        if pr:
            reads = [k for k in reads if k not in pr]
            writes = list(writes) + pr
        self._deps(q, reads, writes)
        ins = fn(q.eng)
        if inc:
            q.count += 1
            ins.then_inc(q.sem, 1)
            ev = (q.sem, q.count)
        else:
            ev = (q.sem, q.count + 1)
        self._commit(ev, reads, writes)
        return ins

    def dma(self, q, out, in_, reads=(), writes=(), slow=False):
        if self._tick():
            return None
        self._deps(q, reads, writes)
        i = q.dnext
        q.dnext = (q.dnext + 1) % len(q.dsems)
        sem = q.dsems[i]
        q.wait((sem, q.dvals[i]))
        ins = q.eng.dma_start(out=out, in_=in_, allow_slow_non_contiguous=True) if slow else q.eng.dma_start(out=out, in_=in_)
        q.dvals[i] += 16
        ins.then_inc(sem, 16)
        self._commit((sem, q.dvals[i]), reads, writes)
        return ins

    def barrier(self):
        evs = []
        for q in self.qs:
            if q.count:
                evs.append((q.sem, q.count))
            for s, v in zip(q.dsems, q.dvals):
                if v:
                    evs.append((s, v))
        for q in self.qs:
            for ev in evs:
                q.wait(ev)
        self.bufs = {}


def build_program(stop_after=99, only_attn=False, nheads=16, ngroups=3, dbg=0, limit=None):
    nc = bass.Bass("TRN2", target_bir_lowering=False)

    TINY = {"w_in", "w_out", "wg", "wu", "wd", "mwg", "mwu", "mwd", "wrT", "w_ao"} if only_attn else set()

    def din(name, shape, dt=F32):
        if name in TINY:
            shape = [1, 1]
        return nc.dram_tensor(name, list(shape), dt, kind="ExternalInput").ap()

    def dout(name, shape, dt=F32):
        return nc.dram_tensor(name, list(shape), dt, kind="ExternalOutput").ap()

    def dscr(name, shape, dt):
        return nc.dram_tensor(name, list(shape), dt).ap()

    xs = din("xs", [T, D])
    cst = din("cst", [128, 768])
    selv = din("selv", [128, 2])
    g_mix0 = din("g_mix0", [128, D])
    g_ffn0 = din("g_ffn0", [128, D])
    g_kv = din("g_kv", [128, D])
    g_mix1 = din("g_mix1", [128, D])
    g_ffn1 = din("g_ffn1", [128, D])
    g_fin = din("g_fin", [128, D])
    g_on = din("g_on", [128, D])
    w_in = din("w_in", [D, 6152])
    bgate = din("bgate", [1, 8])
    w_out = din("w_out", [D, D])
    w_kv = din("w_kv", [D, 12288])
    w_q = din("w_q", [D, 6144])
    w_ao = din("w_ao", [D, D])
    wg = din("wg", [D, DFF])
    wu = din("wu", [D, DFF])
    wd = din("wd", [DFF, D])
    wrT = din("wrT", [128, NE * D])
    brr = din("brr", [128, NE])
    mwg = din("mwg", [NE, D, DFE])
    mwu = din("mwu", [NE, D, DFE])
    mwd = din("mwd", [NE, DFE, D])
    C0 = din("C0", [NH, DK, DV])
    n0 = din("n0", [NH, DK])
    m0 = din("m0", [1, NH])
    caches = [din("ck128", [128, 2, 16, 128]), din("ck512", [512, 2, 16, 128]), din("ck2048", [2048, 2, 16, 128])]

    y = dout("y", [TM, D])
    pC = dout("pC", [NH, DK, DV])
    pn = dout("pn", [NH, DK])
    pm = dout("pm", [1, NH])
    sC = dout("sC", [NH, DK, DV])
    sn = dout("sn", [NH, DK])
    sm = dout("sm", [1, NH])
    pkv = [dout("pkv0", [128, 2, 16, 128]), dout("pkv1", [512, 2, 16, 128]), dout("pkv2", [2048, 2, 16, 128])]
    skv = dout("skv", [3, 2, 16, 128])

    h = dscr("h_res", [T, D], F32)
    hT = dscr("hT", [D, T], BF16)
    qTd = dscr("qTd", [3 * D, T], BF16)
    oT = dscr("oT", [D, T], BF16)
    hm = dscr("hm", [TM, D], F32)

    with ExitStack() as es0:
        Tk = Tracker(nc, es0)
        pe, act, dve, pool, sp = Tk.pe, Tk.act, Tk.dve, Tk.pool, Tk.sp
        op, dma = Tk.op, Tk.dma

        def sbt(es, name, shape, dt):
            return es.enter_context(nc.sbuf_tensor(name, list(shape), dt))

        pA = [es0.enter_context(nc.psum_tensor(f"pA{i}", [128, 512], F32)) for i in range(2)]
        pBk = [es0.enter_context(nc.psum_tensor(f"pB{i}", [128, 512], F32)) for i in range(2)]
        pCk = [es0.enter_context(nc.psum_tensor(f"pC{i}", [128, 512], F32)) for i in range(2)]
        pTk = [es0.enter_context(nc.psum_tensor(f"pT{i}", [128, 8, 128], BF16)) for i in range(2)]

        cf = sbt(es0, "cf", [128, 768], F32)
        idb = sbt(es0, "idb", [128, 128], BF16)
        onb = sbt(es0, "onb", [128, 128], BF16)
        mcur_b = sbt(es0, "mcur_b", [128, 128], BF16)
        mprev_b = sbt(es0, "mprev_b", [128, 128], BF16)
        dma(sp, cf[:], cst, writes=["cf"])
        op(dve, lambda e: e.tensor_copy(idb[:], cf[:, 0:128]), reads=["cf"], writes=["idb"])
        op(dve, lambda e: e.tensor_copy(onb[:], cf[:, 128:256]), reads=["cf"], writes=["onb"])
        op(dve, lambda e: e.tensor_copy(mcur_b[:], cf[:, 256:384]), reads=["cf"], writes=["mcur_b"])
        op(dve, lambda e: e.tensor_copy(mprev_b[:], cf[:, 384:512]), reads=["cf"], writes=["mprev_b"])
        onf = cf[:, 128:256]
        mcur_f = cf[:, 512:640]
        CONST = ["cf", "idb", "onb", "mcur_b", "mprev_b"]

        def const_reset():
            pass

        rr = {"ev": 0}

        def evac_eng():
            rr["ev"] += 1
            return act if rr["ev"] % 2 else dve

        def copy_op(q, out, in_, reads, writes):
            if q is act:
                return op(act, lambda e: e.copy(out, in_), reads=reads, writes=writes)
            return op(q, lambda e: e.tensor_copy(out, in_), reads=reads, writes=writes)

        def norm_stage(es, src, dsts, router=None, tag="n", tiles=None):
            xin = [sbt(es, f"{tag}_xin{i}", [128, D], F32) for i in range(2)]
            junk = sbt(es, f"{tag}_junk", [128, D], BF16)
            st = sbt(es, f"{tag}_st", [128, 4], F32)
            gains = []
            for j, (gd, _, _) in enumerate(dsts):
                gt = sbt(es, f"{tag}_g{j}", [128, D], F32)
                dma(sp, gt[:], gd, writes=[f"{tag}_g{j}"])
                gains.append(gt)
            xnb = [sbt(es, f"{tag}_xnb{i}", [128, D], BF16) for i in range(2)]
            if router is not None:
                xnf = sbt(es, f"{tag}_xnf", [128, D], F32)
                jf = sbt(es, f"{tag}_jf", [128, D], F32)
                wr_sb = sbt(es, f"{tag}_wr", [128, NE * D], F32)
                br_sb = sbt(es, f"{tag}_br", [128, NE], F32)
                lg = sbt(es, f"{tag}_lg", [128, NE], F32)
                rt = sbt(es, f"{tag}_rt", [128, 4 * NE], F32)
                dma(sp, wr_sb[:], wrT, writes=["wr_sb"])
                dma(sp, br_sb[:], brr, writes=["br_sb"])
                gates_sb = router
            cnt = 0
            for (ti, t0, nt) in (tiles or TILES):
                x_ = xin[ti % 2]
                xk = f"{tag}_xin{ti % 2}"
                dma(sp, x_[0:nt, :], src[t0:t0 + nt, :], writes=[xk])
                op(act, lambda e: e.activation(junk[0:nt, :], x_[0:nt, :], AF.Square, accum_out=st[0:nt, 0:1]),
                   reads=[xk], writes=[f"{tag}_junk", f"{tag}_st"])
                op(dve, lambda e: e.tensor_scalar(st[0:nt, 1:2], st[0:nt, 0:1], 1.0 / D, EPS, ALU.mult, ALU.add),
                   reads=[f"{tag}_st"], writes=[f"{tag}_st"])
                op(act, lambda e: e.activation(st[0:nt, 2:3], st[0:nt, 1:2], AF.Sqrt), reads=[f"{tag}_st"], writes=[f"{tag}_st"])
                op(dve, lambda e: e.reciprocal(st[0:nt, 3:4], st[0:nt, 2:3]), reads=[f"{tag}_st"], writes=[f"{tag}_st"])
                for j, (_, dstT, dkey) in enumerate(dsts):
                    xb = xnb[cnt % 2]
                    bk = f"{tag}_xnb{cnt % 2}"
                    cnt += 1
                    if router is not None:
                        op(dve, lambda e: e.scalar_tensor_tensor(xnf[0:nt, :], x_[0:nt, :], st[0:nt, 3:4], gains[j][0:nt, :], ALU.mult, ALU.mult),
                           reads=[xk, f"{tag}_st", f"{tag}_g{j}"], writes=["xnf"])
                        op(act, lambda e: e.copy(xb[0:nt, :], xnf[0:nt, :]), reads=["xnf"], writes=[bk])
                    else:
                        op(dve, lambda e: e.scalar_tensor_tensor(xb[0:nt, :], x_[0:nt, :], st[0:nt, 3:4], gains[j][0:nt, :], ALU.mult, ALU.mult),
                           reads=[xk, f"{tag}_st", f"{tag}_g{j}"], writes=[bk])
                    for half in range(2):
                        pt = pTk[half]
                        pk = f"pT{half}"
                        for c in range(8):
                            cc = half * 8 + c
                            op(pe, lambda e: e.transpose(pt[:, c, 0:nt], xb[0:nt, cc * 128:(cc + 1) * 128], idb[0:nt, 0:nt]),
                               reads=[bk, "idb"], writes=[pk], inc=(c == 7))
                        copy_op(evac_eng(), dstT[:, half * 8:(half + 1) * 8, t0:t0 + nt], pt[:, :, 0:nt], [pk], [dkey])
                if router is not None:
                    for e_ in range(NE):
                        op(dve, lambda e: e.tensor_tensor(jf[0:nt, :], xnf[0:nt, :], wr_sb[0:nt, e_ * D:(e_ + 1) * D], ALU.mult),
                           reads=["xnf", "wr_sb"], writes=["jf"])
                        op(dve, lambda e: e.reduce_sum(lg[0:nt, e_:e_ + 1], jf[0:nt, :], AX.X), reads=["jf"], writes=["lg"])
                    op(dve, lambda e: e.tensor_tensor(lg[0:nt, :], lg[0:nt, :], br_sb[0:nt, :], ALU.add), reads=["lg", "br_sb"], writes=["lg"])
                    op(dve, lambda e: e.reduce_max(rt[0:nt, 0:1], lg[0:nt, :], AX.X), reads=["lg"], writes=["rt"])
                    op(dve, lambda e: e.tensor_scalar(rt[0:nt, 8:16], lg[0:nt, :], rt[0:nt, 0:1], -1e30, ALU.is_equal, ALU.mult), reads=["lg", "rt"], writes=["rt"])
                    op(dve, lambda e: e.tensor_tensor(rt[0:nt, 8:16], rt[0:nt, 8:16], lg[0:nt, :], ALU.add), reads=["lg", "rt"], writes=["rt"])
                    op(dve, lambda e: e.reduce_max(rt[0:nt, 1:2], rt[0:nt, 8:16], AX.X), reads=["rt"], writes=["rt"])
                    op(dve, lambda e: e.tensor_scalar(rt[0:nt, 16:24], lg[0:nt, :], rt[0:nt, 1:2], None, ALU.is_ge), reads=["lg", "rt"], writes=["rt"])
                    op(dve, lambda e: e.tensor_scalar(rt[0:nt, 2:3], rt[0:nt, 0:1], -1.0, None, ALU.mult), reads=["rt"], writes=["rt"])
                    op(act, lambda e: e.activation(rt[0:nt, 24:32], lg[0:nt, :], AF.Exp, bias=rt[0:nt, 2:3]), reads=["lg", "rt"], writes=["rt"])
                    op(dve, lambda e: e.tensor_tensor(rt[0:nt, 24:32], rt[0:nt, 24:32], rt[0:nt, 16:24], ALU.mult), reads=["rt"], writes=["rt"])
                    op(dve, lambda e: e.reduce_sum(rt[0:nt, 3:4], rt[0:nt, 24:32], AX.X), reads=["rt"], writes=["rt"])
                    op(dve, lambda e: e.reciprocal(rt[0:nt, 4:5], rt[0:nt, 3:4]), reads=["rt"], writes=["rt"])
                    op(dve, lambda e: e.tensor_scalar(gates_sb[0:nt, ti, :], rt[0:nt, 24:32], rt[0:nt, 4:5], None, ALU.mult), reads=["rt"], writes=["gates"])

        def outproj(es, aT, akey, KC, W_ap, res_in, res_out, cbw, gates_sb=None, gcol=None, tag="o", tiles=None, nwb=2):
            ncb = D // cbw
            wb = [sbt(es, f"{tag}_wb{i}", [128, KC, cbw], BF16) for i in range(nwb)]
            rin = [sbt(es, f"{tag}_rin{i}", [128, cbw], F32) for i in range(3)]
            Wv = W_ap.rearrange("(c p) m -> p c m", p=128)
            k = 0
            for cb in range(ncb):
                w_ = wb[cb % nwb]
                wk_ = f"{tag}_wb{cb % nwb}"
                dma(pool, w_[:], Wv[:, :, cb * cbw:(cb + 1) * cbw], writes=[wk_])
                for (ti, t0, nt) in (tiles or TILES):
                    pp = pA[k % 2]
                    pk = f"pA{k % 2}"
                    r_ = rin[k % 3]
                    rk = f"{tag}_rin{k % 3}"
                    k += 1
                    dma(sp, r_[0:nt, :], res_in[t0:t0 + nt, cb * cbw:(cb + 1) * cbw], reads=[("hres", ti, cb)] if (res_in is h or res_in is hm) else [], writes=[rk])
                    for kc in range(KC):
                        op(pe, lambda e: e.matmul(pp[0:nt, 0:cbw], aT[:, kc, t0:t0 + nt], w_[:, kc, :], start=(kc == 0), stop=(kc == KC - 1)),
                           reads=[akey, wk_], writes=[pk], inc=(kc == KC - 1))
                    if gates_sb is None:
                        op(dve, lambda e: e.tensor_tensor(r_[0:nt, :], r_[0:nt, :], pp[0:nt, 0:cbw], ALU.add), reads=[pk, rk], writes=[rk])
                    else:
                        op(dve, lambda e: e.scalar_tensor_tensor(r_[0:nt, :], pp[0:nt, 0:cbw], gates_sb[0:nt, ti, gcol:gcol + 1], r_[0:nt, :], ALU.mult, ALU.add),
                           reads=[pk, rk, "gates"], writes=[rk])
                    dma(sp, res_out[t0:t0 + nt, cb * cbw:(cb + 1) * cbw], r_[0:nt, :], reads=[rk], writes=[("hres", ti, cb)])

        def ffn_block(es, xT, xkey, Wg_ap, Wu_ap, Wd_ap, gates_sb=None, gcol=None, tag="f", tiles=None, btiles=None, Tn=T, res=None, nbuf=2, nwb=2):
            NMC = DFE // 128
            actT = sbt(es, f"{tag}_actT", [128, NMC, Tn], BF16)
            wgc = [sbt(es, f"{tag}_wg{i}", [128, 16, 128], BF16) for i in range(nbuf)]
            wuc = [sbt(es, f"{tag}_wu{i}", [128, 16, 128], BF16) for i in range(nbuf)]
            sg = [sbt(es, f"{tag}_sg{i}", [128, 512], F32) for i in range(2)]
            Wgv = Wg_ap.rearrange("(c p) m -> p c m", p=128)
            Wuv = Wu_ap.rearrange("(c p) m -> p c m", p=128)
            k = 0
            for mc in range(NMC):
                g_ = wgc[mc % nbuf]
                u_ = wuc[mc % nbuf]
                gk = f"{tag}_wg{mc % nbuf}"
                uk = f"{tag}_wu{mc % nbuf}"
                dma(pool, g_[:], Wgv[:, :, mc * 128:(mc + 1) * 128], writes=[gk])
                dma(pool, u_[:], Wuv[:, :, mc * 128:(mc + 1) * 128], writes=[uk])
                for (bi, t0, nt) in (btiles or BTILES):
                    pg = pBk[k % 2]
                    pu = pCk[k % 2]
                    pgk = f"pB{k % 2}"
                    puk = f"pC{k % 2}"
                    s_ = sg[k % 2]
                    sk = f"{tag}_sg{k % 2}"
                    k += 1
                    for kc in range(16):
                        op(pe, lambda e: e.matmul(pg[:, 0:nt], g_[:, kc, :], xT[:, kc, t0:t0 + nt], start=(kc == 0), stop=(kc == 15)),
                           reads=[gk, xkey], writes=[pgk], inc=(kc == 15))
                    for kc in range(16):
                        op(pe, lambda e: e.matmul(pu[:, 0:nt], u_[:, kc, :], xT[:, kc, t0:t0 + nt], start=(kc == 0), stop=(kc == 15)),
                           reads=[uk, xkey], writes=[puk], inc=(kc == 15))
                    op(act, lambda e: e.activation(s_[:, 0:nt], pg[:, 0:nt], AF.Silu), reads=[pgk], writes=[sk])
                    op(dve, lambda e: e.tensor_tensor(actT[:, mc, t0:t0 + nt], s_[:, 0:nt], pu[:, 0:nt], ALU.mult), reads=[sk, puk], writes=[f"{tag}_actT"])
            outproj(es, actT, f"{tag}_actT", NMC, Wd_ap, res if res is not None else h, res if res is not None else h, 256, gates_sb=gates_sb, gcol=gcol, tag=tag + "d", tiles=tiles, nwb=nwb)

        with ExitStack() as esA:
            if not only_attn:
              xn0T = sbt(esA, "xn0T", [128, 16, T], BF16)
              with ExitStack() as es:
                norm_stage(es, xs, [(g_mix0, xn0T, "xn0T")], tag="n0")
                Tk.barrier()
            if stop_after >= 2 and not only_attn:
              with ExitStack() as es:
                Wh = sbt(es, "Wh", [128, 16, 1536], BF16)
                Wig = sbt(es, "Wig", [128, 16, 128], BF16)
                Wlf = sbt(es, "Wlf", [128, 16, 128], BF16)
                R = [sbt(es, f"R{i}", [128, T], F32) for i in range(4)]
                Z = sbt(es, "Zrow", [128, T], F32)
                bcol = sbt(es, "bcol", [128, 4], F32)
                mfin = sbt(es, "mfin", [128, 4], F32)
                gon = sbt(es, "gon", [128, D], F32)
                Cf = sbt(es, "Cf", [128, 2, 512], F32)
                Cb = sbt(es, "Cb", [128, 2, 512], BF16)
                nf = sbt(es, "nf", [128, 2], F32)
                nb = sbt(es, "nb", [128, 2], BF16)
                w_inv = w_in.rearrange("(c p) m -> p c m", p=128)
                dma(sp, gon[:], g_on, writes=["gon"])
                qk = [sbt(es, f"qk{i}", [128, 512], BF16) for i in range(2)]
                qw = [sbt(es, f"qw{i}", [128, 256], BF16) for i in range(2)]
                vs_ = [sbt(es, f"vs{i}", [128, 512], BF16) for i in range(2)]
                so = [sbt(es, f"so{i}", [128, 512], F32) for i in range(2)]
                cols = [sbt(es, f"cols{i}", [128, 8], F32) for i in range(2)]
                qkT = [sbt(es, f"qkT{i}", [128, 6, 128], BF16) for i in range(2)]
                tmpf2 = [sbt(es, f"tmpf{i}", [128, 128], F32) for i in range(2)]
                WT2 = [sbt(es, f"WT{i}", [128, 128], F32) for i in range(2)]
                STb2 = [sbt(es, f"STb{i}", [128, 128], BF16) for i in range(2)]
                kw2 = [sbt(es, f"kw{i}", [128, 256], BF16) for i in range(2)]
                hf2 = [sbt(es, f"hf{i}", [128, 512], F32) for i in range(2)]
                hj2 = [sbt(es, f"hj{i}", [128, 512], BF16) for i in range(2)]
                hg2 = [sbt(es, f"hg{i}", [128, 512], BF16) for i in range(2)]
                hst2 = [sbt(es, f"hst{i}", [128, 8], F32) for i in range(2)]
                hTs = [sbt(es, f"hTs{i}", [128, 4, 128], BF16) for i in range(2)]
                hTv = hT.rearrange("(c p) t -> p c t", p=128)
                itn = 0
                for hgrp in ([0, 1, 2], [3]):
                    op(pool, lambda e: e.memset(Wig[:], 0.0), writes=["Wig"])
                    op(pool, lambda e: e.memset(Wlf[:], 0.0), writes=["Wlf"])
                    op(dve, lambda e: e.memset(bcol[:], 0.0), writes=["bcol"])
                    op(dve, lambda e: e.memset(Z[:], 0.0), writes=["Z"])
                    for i in range(4):
                        op(dve, lambda e: e.memset(R[i][:], 0.0), writes=[f"R{i}"])
                    gbase = 2 * NH * DK + 2 * NH * DV
                    for hh in hgrp:
                        dma(pool, Wig[:, :, 32 * hgrp.index(hh):32 * hgrp.index(hh) + 1], w_inv[:, :, gbase + hh:gbase + hh + 1], writes=["Wig"], slow=True)
                        dma(pool, Wlf[:, :, 32 * hgrp.index(hh):32 * hgrp.index(hh) + 1], w_inv[:, :, gbase + NH + hh:gbase + NH + hh + 1], writes=["Wlf"], slow=True)
                        dma(sp, bcol[32 * hgrp.index(hh):32 * hgrp.index(hh) + 1, 0:1], bgate[0:1, hh:hh + 1], writes=["bcol"])
                        dma(sp, bcol[32 * hgrp.index(hh):32 * hgrp.index(hh) + 1, 1:2], bgate[0:1, NH + hh:NH + hh + 1], writes=["bcol"])
                        dma(sp, bcol[32 * hgrp.index(hh):32 * hgrp.index(hh) + 1, 2:3], m0[0:1, hh:hh + 1], writes=["bcol"])
                    op(dve, lambda e: e.tensor_scalar(bcol[:, 0:2], bcol[:, 0:2], 1.0 / 15.0, None, ALU.mult), reads=["bcol"], writes=["bcol"])
                    for (bi, t0, nt) in BTILES:
                        for (Wt, wk_, pp, pk, col, Rd, rk) in ((Wig, "Wig", pA[0], "pA0", 0, R[0], "R0"), (Wlf, "Wlf", pA[1], "pA1", 1, R[1], "R1")):
                            for kc in range(16):
                                op(pe, lambda e: e.matmul(pp[:, 0:nt], Wt[:, kc, :], xn0T[:, kc, t0:t0 + nt], start=(kc == 0), stop=(kc == 15)),
                                   reads=[wk_, "xn0T"], writes=[pk], inc=(kc == 15))
                            op(act, lambda e: e.activation(Rd[:, t0:t0 + nt], pp[:, 0:nt], AF.Tanh, bias=bcol[:, col:col + 1], scale=1.0 / 15.0),
                               reads=[pk, "bcol"], writes=[rk])
                    op(dve, lambda e: e.tensor_scalar(R[0][:], R[0][:], 15.0, None, ALU.mult), reads=["R0"], writes=["R0"])
                    op(act, lambda e: e.activation(R[1][:], R[1][:], AF.Exp, scale=-15.0), reads=["R1"], writes=["R1"])
                    op(act, lambda e: e.activation(R[1][:], R[1][:], AF.Ln, bias=1.0), reads=["R1"], writes=["R1"])
                    op(dve, lambda e: e.tensor_scalar(R[1][:], R[1][:], -1.0, None, ALU.mult), reads=["R1"], writes=["R1"])
                    op(dve, lambda e: e.tensor_tensor_scan(R[2][:, 0:S], R[1][:, 0:S], Z[:, 0:S], 0.0, ALU.add, ALU.add), reads=["R1", "Z"], writes=["R2"])
                    op(dve, lambda e: e.tensor_copy(R[2][:, S:T], R[1][:, S:T]), reads=["R1"], writes=["R2"])
                    op(dve, lambda e: e.tensor_tensor_scan(R[3][:, 0:S], R[1][:, 0:S], R[0][:, 0:S], 0.0, ALU.add, ALU.max), reads=["R1", "R0"], writes=["R3"])
                    op(dve, lambda e: e.scalar_tensor_tensor(R[3][:, S:T], R[1][:, S:T], bcol[:, 2:3], R[0][:, S:T], ALU.add, ALU.max), reads=["R1", "R0", "bcol"], writes=["R3"])
                    op(dve, lambda e: e.tensor_copy(mfin[:, 0:1], R[3][:, S - 1:S]), reads=["R3"], writes=["mfin"])
                    op(dve, lambda e: e.tensor_copy(mfin[:, 1:2], R[3][:, S:T]), reads=["R3"], writes=["mfin"])
                    op(dve, lambda e: e.tensor_tensor(R[1][:], R[2][:], R[3][:], ALU.subtract), reads=["R2", "R3"], writes=["R1"])
                    op(dve, lambda e: e.tensor_tensor(R[2][:], R[0][:], R[2][:], ALU.subtract), reads=["R0", "R2"], writes=["R2"])
                    op(act, lambda e: e.activation(R[3][:], R[3][:], AF.Exp, scale=-1.0), reads=["R3"], writes=["R3"])
                    op(dve, lambda e: e.tensor_copy(R[0][:, 0:128], R[1][:, 0:128]), reads=["R1"], writes=["R0"])
                    for i in range(1, 16):
                        op(dve, lambda e: e.tensor_scalar(R[0][:, i * 128:(i + 1) * 128], R[1][:, i * 128:(i + 1) * 128], R[1][:, i * 128 - 1:i * 128], None, ALU.subtract),
                           reads=["R1"], writes=["R0"])
                    op(dve, lambda e: e.tensor_scalar(R[0][:, S:T], R[1][:, S:T], bcol[:, 2:3], None, ALU.add), reads=["R1", "bcol"], writes=["R0"])
                    op(act, lambda e: e.activation(R[0][:], R[0][:], AF.Exp), reads=["R0"], writes=["R0"])
                    Rwi, Ru, Rc, Rem = R[0], R[1], R[2], R[3]
                    for hh in hgrp:
                        dma(sp, pm[0:1, hh:hh + 1], mfin[32 * hgrp.index(hh):32 * hgrp.index(hh) + 1, 0:1], reads=["mfin"], writes=["o_pm"])
                        dma(sp, sm[0:1, hh:hh + 1], mfin[32 * hgrp.index(hh):32 * hgrp.index(hh) + 1, 1:2], reads=["mfin"], writes=["o_sm"])

                    for hh in hgrp:
                        r0 = 32 * hgrp.index(hh)
                        segs = ((hh * DK, 0, DK), (NH * DK + hh * DK, DK, DK), (2 * NH * DK + hh * DV, 2 * DK, DV), (2 * NH * DK + NH * DV + hh * DV, 2 * DK + DV, DV))
                        for (src0, dst0, wdt) in segs:
                            dma(pool, Wh[:, :, dst0:dst0 + wdt], w_inv[:, :, src0:src0 + wdt], writes=["Wh"])
                        op(dve, lambda e: e.memset(Cf[:], 0.0), writes=["Cf"])
                        op(pool, lambda e: e.memset(Cb[:], 0.0), writes=["Cb"])
                        op(dve, lambda e: e.memset(nf[:], 0.0), writes=["nf"])
                        op(pool, lambda e: e.memset(nb[:], 0.0), writes=["nb"])
                        for (ti, t0, nt) in TILES:
                            if ti == 16:
                                dma(sp, pC[hh].rearrange("(c p) e -> p c e", p=128), Cf[:], reads=["Cf"], writes=["o_pC"])
                                for dc in range(2):
                                    dma(sp, pn[hh:hh + 1, dc * 128:(dc + 1) * 128].rearrange("o p -> p o"), nf[:, dc:dc + 1], reads=["nf"], writes=["o_pn"], slow=True)
                                dma(sp, Cf[:], C0[hh].rearrange("(c p) e -> p c e", p=128), writes=["Cf"])
                                for dc in range(2):
                                    dma(sp, nf[:, dc:dc + 1], n0[hh:hh + 1, dc * 128:(dc + 1) * 128].rearrange("o p -> p o"), writes=["nf"], slow=True)
                                op(act, lambda e: e.copy(Cb[:], Cf[:]), reads=["Cf"], writes=["Cb"])
                                op(dve, lambda e: e.tensor_copy(nb[:], nf[:]), reads=["nf"], writes=["nb"])
                            sl = itn % 2
                            itn += 1
                            qk_, qw_, v_, so_, cl_, qT_ = qk[sl], qw[sl], vs_[sl], so[sl], cols[sl], qkT[sl]
                            tmpf, WT, STb, kw, hf, hj, hg, hst = tmpf2[sl], WT2[sl], STb2[sl], kw2[sl], hf2[sl], hj2[sl], hg2[sl], hst2[sl]
                            K_tmpf, K_WT, K_STb, K_kw, K_hf, K_hj, K_hg, K_hst = f"tmpf{sl}", f"WT{sl}", f"STb{sl}", f"kw{sl}", f"hf{sl}", f"hj{sl}", f"hg{sl}", f"hst{sl}"
                            kq, kqw, kv_, kso, kcl, kqT = f"qk{sl}", f"qw{sl}", f"vs{sl}", f"so{sl}", f"cols{sl}", f"qkT{sl}"
                            pq, pv, po_ = pA[sl], pBk[sl], pCk[sl]
                            kpq, kpv, kpo = f"pA{sl}", f"pB{sl}", f"pC{sl}"
                            for (pp, pk, c0) in ((pq, kpq, 0), (pv, kpv, 512), (po_, kpo, 1024)):
                                for kc in range(16):
                                    op(pe, lambda e: e.matmul(pp[0:nt, :], xn0T[:, kc, t0:t0 + nt], Wh[:, kc, c0:c0 + 512], start=(kc == 0), stop=(kc == 15)),
                                       reads=["xn0T", "Wh"], writes=[pk], inc=(kc == 15))
                            pD = pTk
                            psm = pCk[1 - sl] if False else None
                            px = pCk[1 - sl]
                            kpx = f"pC{1 - sl}"
                            for j, Rr in enumerate((Rc, Rwi, Rem)):
                                rkey = ("R2", "R0", "R3")[j]
                                op(pe, lambda e: e.matmul(px[0:nt, j:j + 1], Rr[r0:r0 + 1, t0:t0 + nt], onf[r0:r0 + 1, 0:1], start=True, stop=True),
                                   reads=[rkey, "cf"], writes=[kpx], inc=False)
                            op(pe, lambda e: e.matmul(px[:, 4:5], onf[r0:r0 + 1, 0:128], Rwi[r0:r0 + 1, t0 + nt - 1:t0 + nt], start=True, stop=True),
                               reads=["R0", "cf"], writes=[kpx], inc=False)
                            op(pe, lambda e: e.matmul(px[0:nt, 128:128 + nt], onf[r0:r0 + 1, 0:nt], Ru[r0:r0 + 1, t0:t0 + nt], start=True, stop=True),
                               reads=["R1", "cf"], writes=[kpx])
                            op(act, lambda e: e.copy(cl_[0:nt, 0:3], px[0:nt, 0:3]), reads=[kpx], writes=[kcl])
                            op(act, lambda e: e.copy(cl_[:, 4:5], px[:, 4:5]), reads=[kpx], writes=[kcl])
                            op(dve, lambda e: e.tensor_tensor(tmpf[0:nt, 0:nt], px[0:nt, 128:128 + nt], mcur_f[0:nt, 0:nt], ALU.add), reads=[kpx, "cf"], writes=[K_tmpf])
                            op(act, lambda e: e.activation(WT[0:nt, 0:nt], tmpf[0:nt, 0:nt], AF.Exp, bias=cl_[0:nt, 0:1]), reads=[K_tmpf, kcl], writes=[K_WT])
                            op(act, lambda e: e.copy(qk_[0:nt, 0:256], pq[0:nt, 0:256]), reads=[kpq], writes=[kq])
                            op(dve, lambda e: e.tensor_scalar(qk_[0:nt, 256:512], pq[0:nt, 256:512], DK ** -0.5, None, ALU.mult), reads=[kpq], writes=[kq])
                            op(dve, lambda e: e.tensor_scalar(qw_[0:nt, :], pq[0:nt, 0:256], cl_[0:nt, 1:2], None, ALU.mult), reads=[kpq, kcl], writes=[kqw])
                            op(act, lambda e: e.copy(v_[0:nt, :], pv[0:nt, :]), reads=[kpv], writes=[kv_])
                            op(act, lambda e: e.activation(so_[0:nt, :], po_[0:nt, :], AF.Sigmoid), reads=[kpo], writes=[kso])
                            pt = pTk[sl]
                            kpt = f"pT{sl}"
                            srcs = ((qk_, kq, 0), (qk_, kq, 128), (qw_, kqw, 0), (qw_, kqw, 128), (qk_, kq, 256), (qk_, kq, 384))
                            for j, (sv, skk, c0) in enumerate(srcs):
                                op(pe, lambda e: e.transpose(pt[:, j, 0:nt], sv[0:nt, c0:c0 + 128], idb[0:nt, 0:nt]), reads=[skk, "idb"], writes=[kpt], inc=(j == 5))
                            op(dve, lambda e: e.tensor_copy(qT_[:, :, 0:nt], pt[:, 0:6, 0:nt]), reads=[kpt], writes=[kqT])
                            for dc in range(2):
                                op(pe, lambda e: e.matmul(px[0:nt, 256:256 + nt], qT_[:, 4 + dc, 0:nt], qT_[:, dc, 0:nt], start=(dc == 0), stop=(dc == 1)),
                                   reads=[kqT], writes=[kpx], inc=(dc == 1))
                            op(dve, lambda e: e.tensor_tensor(STb[0:nt, 0:nt], px[0:nt, 256:256 + nt], WT[0:nt, 0:nt], ALU.mult), reads=[kpx, K_WT], writes=[K_STb])
                            op(dve, lambda e: e.tensor_scalar(kw[0:nt, :], qk_[0:nt, 256:512], WT[0:nt, nt - 1:nt], None, ALU.mult), reads=[kq, K_WT], writes=[K_kw])
                            pnum = pq
                            op(pe, lambda e: e.matmul(pnum[0:nt, :], STb[0:nt, 0:nt], v_[0:nt, :], start=True, stop=False), reads=[K_STb, kv_, kq, kqw], writes=[kpq], inc=False)
                            for dc in range(2):
                                op(pe, lambda e: e.matmul(pnum[0:nt, :], qT_[:, 2 + dc, 0:nt], Cb[:, dc, :], start=False, stop=(dc == 1)), reads=[kqT, "Cb"], writes=[kpq], inc=(dc == 1))
                            op(pe, lambda e: e.matmul(px[0:nt, 8:9], STb[0:nt, 0:nt], onb[0:nt, 0:1], start=True, stop=False), reads=[K_STb, "onb"], writes=[kpx], inc=False)
                            for dc in range(2):
                                op(pe, lambda e: e.matmul(px[0:nt, 8:9], qT_[:, 2 + dc, 0:nt], nb[:, dc:dc + 1], start=False, stop=(dc == 1)), reads=[kqT, "nb"], writes=[kpx], inc=(dc == 1))
                            op(act, lambda e: e.activation(hst[0:nt, 6:7], px[0:nt, 8:9], AF.Abs), reads=[kpx], writes=[K_hst])
                            op(dve, lambda e: e.tensor_tensor(hst[0:nt, 0:1], hst[0:nt, 6:7], cl_[0:nt, 2:3], ALU.max), reads=[K_hst, kcl], writes=[K_hst])
                            op(dve, lambda e: e.reciprocal(hst[0:nt, 1:2], hst[0:nt, 0:1]), reads=[K_hst], writes=[K_hst])
                            op(dve, lambda e: e.tensor_scalar(hf[0:nt, :], pnum[0:nt, :], hst[0:nt, 1:2], None, ALU.mult), reads=[kpq, K_hst], writes=[K_hf])
                            for dc in range(2):
                                pcs = (pv, po_)[dc]
                                kpcs = (kpv, kpo)[dc]
                                op(pe, lambda e: e.matmul(pcs[:, :], kw[0:nt, dc * 128:(dc + 1) * 128], v_[0:nt, :], start=True, stop=True), reads=[K_kw, kv_, kso], writes=[kpcs])
                                op(pe, lambda e: e.matmul(px[:, 12 + dc:13 + dc], kw[0:nt, dc * 128:(dc + 1) * 128], onb[0:nt, 0:1], start=True, stop=True), reads=[K_kw, "onb"], writes=[kpx])
                                op(dve, lambda e: e.scalar_tensor_tensor(Cf[:, dc, :], Cf[:, dc, :], cl_[:, 4:5], pcs[:, :], ALU.mult, ALU.add), reads=[kpcs, kcl, "Cf"], writes=["Cf"])
                            op(dve, lambda e: e.scalar_tensor_tensor(nf[:, :], nf[:, :], cl_[:, 4:5], px[:, 12:14], ALU.mult, ALU.add), reads=[kpx, kcl, "nf"], writes=["nf"])
                            op(act, lambda e: e.copy(Cb[:], Cf[:]), reads=["Cf"], writes=["Cb"])
                            op(dve, lambda e: e.tensor_copy(nb[:], nf[:]), reads=["nf"], writes=["nb"])
                            op(act, lambda e: e.activation(hj[0:nt, :], hf[0:nt, :], AF.Square, accum_out=hst[0:nt, 2:3]), reads=[K_hf], writes=[K_hj, K_hst])
                            op(dve, lambda e: e.tensor_scalar(hst[0:nt, 3:4], hst[0:nt, 2:3], 1.0 / DV, EPS, ALU.mult, ALU.add), reads=[K_hst], writes=[K_hst])
                            op(act, lambda e: e.activation(hst[0:nt, 4:5], hst[0:nt, 3:4], AF.Sqrt), reads=[K_hst], writes=[K_hst])
                            op(dve, lambda e: e.reciprocal(hst[0:nt, 5:6], hst[0:nt, 4:5]), reads=[K_hst], writes=[K_hst])
                            op(dve, lambda e: e.scalar_tensor_tensor(hf[0:nt, :], hf[0:nt, :], hst[0:nt, 5:6], gon[0:nt, hh * DV:(hh + 1) * DV], ALU.mult, ALU.mult), reads=[K_hf, K_hst, "gon"], writes=[K_hf])
                            op(dve, lambda e: e.tensor_tensor(hg[0:nt, :], hf[0:nt, :], so_[0:nt, :], ALU.mult), reads=[K_hf, kso], writes=[K_hg])
                            for j in range(4):
                                op(pe, lambda e: e.transpose(pt[:, j, 0:nt], hg[0:nt, j * 128:(j + 1) * 128], idb[0:nt, 0:nt]), reads=[K_hg, "idb", kqT], writes=[kpt], inc=(j == 3))
                            ht_ = hTs[sl]
                            kht = f"hTs{sl}"
                            op(act, lambda e: e.copy(ht_[:, :, 0:nt], pt[:, 0:4, 0:nt]), reads=[kpt], writes=[kht])
                            dma(sp, hTv[:, hh * 4:(hh + 1) * 4, t0:t0 + nt], ht_[:, :, 0:nt], reads=[kht], writes=[("hT", hh, ti)], slow=(nt == 1))
                        dma(sp, sC[hh].rearrange("(c p) e -> p c e", p=128), Cf[:], reads=["Cf"], writes=["o_sC"])
                        for dc in range(2):
                            dma(sp, sn[hh:hh + 1, dc * 128:(dc + 1) * 128].rearrange("o p -> p o"), nf[:, dc:dc + 1], reads=["nf"], writes=["o_sn"], slow=True)
                Tk.barrier()

        if stop_after >= 3 and not only_attn:
            with ExitStack() as es:
                aT = sbt(es, "aT", [128, 16, T], BF16)
                dma(sp, aT[:], hT.rearrange("(c p) t -> p c t", p=128), writes=["aT"])
                outproj(es, aT, "aT", 16, w_out, xs, h, 512, tag="wo")
                Tk.barrier()
        if stop_after >= 5 and not only_attn:
            with ExitStack() as esA:
                xn1T = sbt(esA, "xn1T", [128, 16, T], BF16)
                with ExitStack() as es:
                    norm_stage(es, h, [(g_ffn0, xn1T, "xn1T")], tag="n1")
                    Tk.barrier()
                for fb in range(2):
                    with ExitStack() as es:
                        ffn_block(es, xn1T, "xn1T", wg[:, fb * DFE:(fb + 1) * DFE], wu[:, fb * DFE:(fb + 1) * DFE], wd[fb * DFE:(fb + 1) * DFE, :], tag=f"f{fb}")
                        Tk.barrier()
        if stop_after >= 8:
            with ExitStack() as esA:
                xkvT = sbt(esA, "xkvT", [128, 16, T], BF16)
                with ExitStack() as esB:
                    xqT = sbt(esB, "xqT", [128, 16, T], BF16)
                    with ExitStack() as es:
                        norm_stage(es, xs if only_attn else h, [(g_kv, xkvT, "xkvT"), (g_mix1, xqT, "xqT")], tag="n2")
                        Tk.barrier()
                    with ExitStack() as es:
                        wqc = [sbt(es, f"wqc{i}", [128, 16, 128], BF16) for i in range(2)]
                        qTs = [sbt(es, f"qTs{i}", [128, T], BF16) for i in range(2)]
                        w_qv = w_q.rearrange("(c p) m -> p c m", p=128)
                        k = 0
                        for mc in range(48):
                            w_ = wqc[mc % 2]
                            wk_ = f"wqc{mc % 2}"
                            q_ = qTs[mc % 2]
                            qk_ = f"qTs{mc % 2}"
                            dma(pool, w_[:], w_qv[:, :, mc * 128:(mc + 1) * 128], writes=[wk_])
                            for (bi, t0, nt) in BTILES:
                                pp = pA[k % 2]
                                pk = f"pA{k % 2}"
                                k += 1
                                for kc in range(16):
                                    op(pe, lambda e: e.matmul(pp[:, 0:nt], w_[:, kc, :], xqT[:, kc, t0:t0 + nt], start=(kc == 0), stop=(kc == 15)),
                                       reads=[wk_, "xqT"], writes=[pk], inc=(kc == 15))
                                copy_op(evac_eng(), q_[:, t0:t0 + nt], pp[:, 0:nt], [pk], [qk_])
                            dma(sp, qTd[mc * 128:(mc + 1) * 128, :], q_[:], reads=[qk_], writes=[("qTd", mc)])
                        Tk.barrier()
                with ExitStack() as es:
                    Oacc = sbt(es, "Oacc", [128, T], F32)
                    Lacc = sbt(es, "Lacc", [128, T], F32)
                    oTh = sbt(es, "oTh", [128, T], BF16)
                    wkv = [sbt(es, f"wkv{i}", [128, 16, 256], BF16) for i in range(2)]
                    qTh = [sbt(es, f"qTh{i}", [128, T], BF16) for i in range(2)]
                    KT = sbt(es, "KT", [128, 16, 128], BF16)
                    Vall = sbt(es, "Vall", [128, 16, 128], BF16)
                    kvf = [sbt(es, f"kvf{i}", [128, 256], F32) for i in range(3)]
                    kvb = [sbt(es, f"kvb{i}", [128, 128], BF16) for i in range(2)]
                    cKV = sbt(es, "cKV", [128, 2, 128], BF16)
                    cKT = sbt(es, "cKT", [128, 128], BF16)
                    KsT = sbt(es, "KsT", [128, 1], BF16)
                    Vs = sbt(es, "Vs", [1, 128], BF16)
                    Pb = [sbt(es, f"Pb{i}", [128, 2, 128], BF16) for i in range(3)]
                    w_kvv = w_kv.rearrange("(c p) m -> p c m", p=128)
                    scale = 128.0 ** -0.5
                    it = 0
                    fcount = 0
                    Tk.limit = limit
                    for hd in range(nheads):
                        op(pool, lambda e: e.memset(Oacc[:], 0.0), writes=["Oacc"])
                        op(pool, lambda e: e.memset(Lacc[:], 0.0), writes=["Lacc"])
                        for g, (window, r) in enumerate(GROUPS[:ngroups]):
                            nbk = 16 // r
                            sl = it % 2
                            it += 1
                            wk_ = wkv[sl]
                            kwk = f"wkv{sl}"
                            q_ = qTh[sl]
                            kq_ = f"qTh{sl}"
                            ck = ((g * 2 + 0) * 16 + hd) * 128
                            cv = ((g * 2 + 1) * 16 + hd) * 128
                            dma(pool, wk_[:, :, 0:128], w_kvv[:, :, ck:ck + 128], writes=[kwk])
                            dma(pool, wk_[:, :, 128:256], w_kvv[:, :, cv:cv + 128], writes=[kwk])
                            dma(sp, q_[:], qTd[(g * 16 + hd) * 128:(g * 16 + hd + 1) * 128, :], reads=[("qTd", g * 16 + hd)], writes=[kq_])
                            if dbg != 12:
                                dma(pool, cKV[:], caches[g][DS(0, 128, r), :, hd, :], writes=["cKV"])
                            for f in range(17 if dbg != 13 else 16):
                                if f < 16:
                                    res, blk = f // nbk, f % nbk
                                    start = res + r * 128 * blk
                                    nt = 128
                                    tok = DS(start, 128, r)
                                else:
                                    nt = 1
                                    tok = DS(S, 1, 1)
                                pp = pA[fcount % 2]
                                pk = f"pA{fcount % 2}"
                                kf_ = kvf[fcount % 3]
                                kkf = f"kvf{fcount % 3}"
                                kb_ = kvb[fcount % 2]
                                kkb = f"kvb{fcount % 2}"
                                pt = pTk[fcount % 2]
                                kpt = f"pT{fcount % 2}"
                                fcount += 1
                                for kc in range(16):
                                    op(pe, lambda e: e.matmul(pp[0:nt, 0:256], xkvT[:, kc, tok], wk_[:, kc, :], start=(kc == 0), stop=(kc == 15)),
                                       reads=["xkvT", kwk], writes=[pk], inc=(kc == 15))
                                op(act, lambda e: e.copy(kf_[0:nt, :], pp[0:nt, 0:256]), reads=[pk], writes=[kkf])
                                op(dve, lambda e: e.tensor_copy(kb_[0:nt, :], pp[0:nt, 0:128]), reads=[pk], writes=[kkb])
                                if f < 16:
                                    op(dve, lambda e: e.tensor_copy(Vall[:, f, :], pp[:, 128:256]), reads=[pk], writes=[("Vall", f)])
                                    op(pe, lambda e: e.transpose(pt[:, 0, :], kb_[:, :], idb[:]), reads=[kkb, "idb"], writes=[kpt])
                                    op(act, lambda e: e.copy(KT[:, f, :], pt[:, 0, :]), reads=[kpt], writes=[("KT", f)])
                                    kview = kf_[:].rearrange("p (a d) -> p a d", a=2)
                                    if dbg == 11:
                                        pass
                                    elif g == 0 and f == 15:
                                        dma(sp, pkv[0][:, :, hd, :], kview, reads=[kkf], writes=["o_pkv"])
                                    elif g == 1 and blk == 3:
                                        dma(sp, pkv[1][DS(res, 128, 4), :, hd, :], kview, reads=[kkf], writes=["o_pkv"])
                                    elif g == 2:
                                        dma(sp, pkv[2][DS(res, 128, 16), :, hd, :], kview, reads=[kkf], writes=["o_pkv"])
                                else:
                                    op(dve, lambda e: e.tensor_copy(Vs[0:1, :], pp[0:1, 128:256]), reads=[pk], writes=["Vs"])
                                    op(pe, lambda e: e.transpose(pt[:, 0, 0:1], kb_[0:1, :], idb[0:1, 0:1]), reads=[kkb, "idb"], writes=[kpt])
                                    op(act, lambda e: e.copy(KsT[:, 0:1], pt[:, 0, 0:1]), reads=[kpt], writes=["KsT"])
                                    if dbg != 11:
                                        dma(sp, skv[g, :, hd, :], kf_[0:1, :].rearrange("p (a d) -> p a d", a=2), reads=[kkf], writes=["o_skv"])
                            if dbg in (1, 11, 12, 13, 14):
                                continue
                            ptc = pTk[fcount % 2]
                            kptc = f"pT{fcount % 2}"
                            op(pe, lambda e: e.transpose(ptc[:, 1, :], cKV[:, 0, :], idb[:]), reads=["cKV", "idb"], writes=[kptc])
                            op(act, lambda e: e.copy(cKT[:], ptc[:, 1, :]), reads=[kptc], writes=["cKT"])
                            for f in range(16 if dbg != 3 else 0):
                                res, blk = f // nbk, f % nbk
                                start = res + r * 128 * blk
                                qv = q_[:, DS(start, 128, r)]
                                keys = ([(f - 1, mprev_b, "mprev_b")] if blk > 0 else []) + [(f, mcur_b, "mcur_b")]
                                ps_ = (pBk[0], pBk[1], pA[0])[f % 3]
                                kps = ("pB0", "pB1", "pA0")[f % 3]
                                po2 = (pCk[0], pCk[1], pA[1])[f % 3]
                                kpo2 = ("pC0", "pC1", "pA1")[f % 3]
                                P_ = Pb[f % 3]
                                kP = f"Pb{f % 3}"
                                nk = len(keys)
                                for s_i, (kfi, mk, mkk) in enumerate(keys):
                                    op(pe, lambda e: e.matmul(ps_[:, s_i * 128:(s_i + 1) * 128], KT[:, kfi, :], qv, start=True, stop=False),
                                       reads=[("KT", kfi), kq_], writes=[kps], inc=False)
                                    op(pe, lambda e: e.matmul(ps_[:, s_i * 128:(s_i + 1) * 128], idb[:], mk[:], start=False, stop=True),
                                       reads=["idb", mkk], writes=[kps], inc=(s_i == nk - 1))
                                op(act, lambda e: e.activation(P_[:, 0:nk, :], ps_[:, 0:nk * 128].rearrange("p (a q) -> p a q", a=nk), AF.Exp, scale=scale),
                                   reads=[kps], writes=[kP])
                                for s_i, (kfi, mk, mkk) in enumerate(keys):
                                    op(pe, lambda e: e.matmul(po2[:, 0:128], Vall[:, kfi, :], P_[:, s_i, :], start=(s_i == 0), stop=(s_i == nk - 1)),
                                       reads=[("Vall", kfi), kP], writes=[kpo2], inc=False)
                                for s_i, (kfi, mk, mkk) in enumerate(keys):
                                    op(pe, lambda e: e.matmul(po2[:, 128:256], onb[:], P_[:, s_i, :], start=(s_i == 0), stop=(s_i == nk - 1)),
                                       reads=["onb", kP], writes=[kpo2], inc=(s_i == nk - 1))
                                op(dve, lambda e: e.tensor_tensor(Oacc[:, DS(start, 128, r)], Oacc[:, DS(start, 128, r)], po2[:, 0:128], ALU.add), reads=[kpo2, "Oacc"], writes=["Oacc"])
                                op(dve, lambda e: e.tensor_tensor(Lacc[:, DS(start, 128, r)], Lacc[:, DS(start, 128, r)], po2[:, 128:256], ALU.add), reads=[kpo2, "Lacc"], writes=["Lacc"])
                            if dbg == 2:
                                continue
                            ps_ = pBk[0]
                            po2 = pCk[0]
                            P_ = Pb[0]
                            qs_ = q_[:, S:T]
                            op(pe, lambda e: e.matmul(ps_[:, 0:1], cKT[:], qs_, start=True, stop=True), reads=["cKT", kq_], writes=["pB0"])
                            op(pe, lambda e: e.matmul(ps_[0:1, 128:129], KsT[:, 0:1], qs_, start=True, stop=True), reads=["KsT", kq_], writes=["pB0"])
                            op(act, lambda e: e.activation(P_[:, 0, 0:1], ps_[:, 0:1], AF.Exp, scale=scale), reads=["pB0"], writes=["Pb0"])
                            op(act, lambda e: e.activation(P_[0:1, 1, 0:1], ps_[0:1, 128:129], AF.Exp, scale=scale), reads=["pB0"], writes=["Pb0"])
                            op(pe, lambda e: e.matmul(po2[:, 0:1], cKV[:, 1, :], P_[:, 0, 0:1], start=True, stop=False), reads=["cKV", "Pb0"], writes=["pC0"], inc=False)
                            op(pe, lambda e: e.matmul(po2[:, 0:1], Vs[0:1, :], P_[0:1, 1, 0:1], start=False, stop=True), reads=["Vs", "Pb0"], writes=["pC0"], inc=False)
                            op(pe, lambda e: e.matmul(po2[:, 128:129], onb[:], P_[:, 0, 0:1], start=True, stop=False), reads=["onb", "Pb0"], writes=["pC0"], inc=False)
                            op(pe, lambda e: e.matmul(po2[:, 128:129], onb[0:1, :], P_[0:1, 1, 0:1], start=False, stop=True), reads=["onb", "Pb0"], writes=["pC0"])
                            op(dve, lambda e: e.tensor_tensor(Oacc[:, S:T], Oacc[:, S:T], po2[:, 0:1], ALU.add), reads=["pC0", "Oacc"], writes=["Oacc"])
                            op(dve, lambda e: e.tensor_tensor(Lacc[:, S:T], Lacc[:, S:T], po2[:, 128:129], ALU.add), reads=["pC0", "Lacc"], writes=["Lacc"])
                        op(dve, lambda e: e.reciprocal(Lacc[:], Lacc[:]), reads=["Lacc"], writes=["Lacc"])
                        op(dve, lambda e: e.tensor_tensor(oTh[:], Oacc[:], Lacc[:], ALU.mult), reads=["Oacc", "Lacc"], writes=["oTh"])
                        dma(sp, oT[hd * 128:(hd + 1) * 128, :], oTh[:], reads=["oTh"], writes=[("oT", hd)])
                    Tk.limit = None
                    Tk.dead = False
                    Tk.barrier()
        if stop_after >= 9:
            with ExitStack() as es:
                aT = sbt(es, "aT2", [128, 16, T], BF16)
                dma(sp, aT[:], oT.rearrange("(c p) t -> p c t", p=128), writes=["aT2"])
                outproj(es, aT, "aT2", 16, w_ao, h, h, 512, tag="ao")
                Tk.barrier()
        if stop_after >= 11:
            with ExitStack() as es:
                selt = sbt(es, "selt", [128, 2], F32)
                lo = [sbt(es, f"blo{i}", [128, D], F32) for i in range(2)]
                hi = [sbt(es, f"bhi{i}", [128, D], F32) for i in range(2)]
                dma(sp, selt[:], selv, writes=["selt"])
                for j in range(8):
                    l_, h_ = lo[j % 2], hi[j % 2]
                    lk, hk = f"blo{j % 2}", f"bhi{j % 2}"
                    dma(sp, l_[:], h[j * 128:(j + 1) * 128, :], writes=[lk])
                    dma(sp, h_[:], h[1024 + j * 128:1024 + (j + 1) * 128, :], writes=[hk])
                    op(dve, lambda e: e.tensor_scalar(l_[:], l_[:], selt[:, 0:1], None, ALU.mult), reads=[lk, "selt"], writes=[lk])
                    op(dve, lambda e: e.scalar_tensor_tensor(l_[:], h_[:], selt[:, 1:2], l_[:], ALU.mult, ALU.add), reads=[hk, lk, "selt"], writes=[lk])
                    dma(sp, hm[j * 128:(j + 1) * 128, :], l_[:], reads=[lk], writes=[("hm", j)])
                dma(sp, lo[0][0:1, :], h[S:T, :], writes=["blo0"])
                dma(sp, hm[1024:1025, :], lo[0][0:1, :], reads=["blo0"], writes=[("hm", 8)])
                Tk.barrier()
            with ExitStack() as esA:
                xn3T = sbt(esA, "xn3T", [128, 16, TM], BF16)
                gates_sb = sbt(esA, "gates", [128, 9, NE], F32)
                with ExitStack() as es:
                    norm_stage(es, hm, [(g_ffn1, xn3T, "xn3T")], router=gates_sb, tag="n3", tiles=MTILES)
                    Tk.barrier()
                for e_ in range(NE):
                    with ExitStack() as es:
                        ffn_block(es, xn3T, "xn3T", mwg[e_], mwu[e_], mwd[e_], gates_sb=gates_sb, gcol=e_, tag=f"m{e_}",
                                  tiles=MTILES, btiles=MBTILES, Tn=TM, res=hm, nbuf=4, nwb=3)
                        Tk.barrier()
        with ExitStack() as es:
            xin = [sbt(es, f"fx{i}", [128, D], F32) for i in range(2)]
            yo = [sbt(es, f"fy{i}", [128, D], F32) for i in range(2)]
            junk = sbt(es, "fjunk", [128, D], BF16)
            st = sbt(es, "fst", [128, 4], F32)
            gt = sbt(es, "fg", [128, D], F32)
            dma(sp, gt[:], g_fin, writes=["fg"])
            src = hm
            for (ti, t0, nt) in MTILES:
                x_ = xin[ti % 2]
                xk = f"fx{ti % 2}"
                y_ = yo[ti % 2]
                yk = f"fy{ti % 2}"
                dma(sp, x_[0:nt, :], src[t0:t0 + nt, :], writes=[xk])
                op(act, lambda e: e.activation(junk[0:nt, :], x_[0:nt, :], AF.Square, accum_out=st[0:nt, 0:1]), reads=[xk], writes=["fjunk", "fst"])
                op(dve, lambda e: e.tensor_scalar(st[0:nt, 1:2], st[0:nt, 0:1], 1.0 / D, EPS, ALU.mult, ALU.add), reads=["fst"], writes=["fst"])
                op(act, lambda e: e.activation(st[0:nt, 2:3], st[0:nt, 1:2], AF.Sqrt), reads=["fst"], writes=["fst"])
                op(dve, lambda e: e.reciprocal(st[0:nt, 3:4], st[0:nt, 2:3]), reads=["fst"], writes=["fst"])
                op(dve, lambda e: e.scalar_tensor_tensor(y_[0:nt, :], x_[0:nt, :], st[0:nt, 3:4], gt[0:nt, :], ALU.mult, ALU.mult), reads=[xk, "fst", "fg"], writes=[yk])
                dma(sp, y[t0:t0 + nt, :], y_[0:nt, :], reads=[yk], writes=["o_y"])
            Tk.barrier()
    return nc


def _consts():
    c = np.zeros((128, 768), np.float32)
    c[:, 0:128] = np.eye(128, dtype=np.float32)
    c[:, 128:256] = 1.0
    k = np.arange(128)[:, None]
    q = np.arange(128)[None, :]
    c[:, 256:384] = np.where(k <= q, 0.0, NEG)
    c[:, 384:512] = np.where(k >= q, 0.0, NEG)
    c[:, 512:640] = np.where(k <= q, 0.0, -1e30)
    return c


_NC_CACHE = {}


def kernel(x_prompt, x_sample, state_mlstm_C, state_mlstm_n, state_mlstm_m, cache_kv_w128, cache_kv_w512,
           cache_kv_w2048, norm_mix, norm_ffn, mlstm_w_in, mlstm_b_gates, mlstm_out_norm, mlstm_w_out, kv_norm,
           w_kv, attn_w_q, attn_w_out, ffn_w_gate, ffn_w_up, ffn_w_down, moe_w_router, moe_b_router, moe_w_gate,
           moe_w_up, moe_w_down, final_norm, _stop_after=99):
    f = lambda a: np.ascontiguousarray(np.asarray(a, dtype=np.float32))
    rep = lambda v: np.ascontiguousarray(np.broadcast_to(np.asarray(v, np.float32).reshape(1, -1), (128, np.asarray(v).size)))
    if _stop_after not in _NC_CACHE:
        _NC_CACHE[_stop_after] = build_program(_stop_after)
    nc = _NC_CACHE[_stop_after]
    shared = {
        "cst": _consts(),
        "g_mix0": rep(norm_mix[0]), "g_ffn0": rep(norm_ffn[0]), "g_kv": rep(kv_norm), "g_mix1": rep(norm_mix[1]),
        "g_ffn1": rep(norm_ffn[1]), "g_fin": rep(final_norm), "g_on": rep(mlstm_out_norm[0]),
        "w_in": f(mlstm_w_in[0]), "bgate": f(mlstm_b_gates).reshape(1, 8), "w_out": f(mlstm_w_out[0]), "w_kv": f(w_kv),
        "w_q": f(attn_w_q[0]), "w_ao": f(attn_w_out[0]), "wg": f(ffn_w_gate[0]), "wu": f(ffn_w_up[0]), "wd": f(ffn_w_down[0]),
        "wrT": rep(np.asarray(moe_w_router[0], np.float32).T.reshape(-1)), "brr": rep(moe_b_router[0]),
        "mwg": f(moe_w_gate[0]), "mwu": f(moe_w_up[0]), "mwd": f(moe_w_down[0]),
    }
    xp = np.asarray(x_prompt, np.float32)
    xsm = np.asarray(x_sample, np.float32)
    in_maps = []
    for c in range(8):
        m = dict(shared)
        m["xs"] = np.ascontiguousarray(np.concatenate([xp[c % 4], xsm[c]], axis=0))
        m["selv"] = np.ascontiguousarray(np.broadcast_to(np.array([[1.0, 0.0]] if c < 4 else [[0.0, 1.0]], np.float32), (128, 2)))
        m["C0"] = f(state_mlstm_C[0, c])
        m["n0"] = f(state_mlstm_n[0, c])
        m["m0"] = f(state_mlstm_m[0, c]).reshape(1, 4)
        m["ck128"] = f(cache_kv_w128[c])
        m["ck512"] = f(cache_kv_w512[c])
        m["ck2048"] = f(cache_kv_w2048[c])
        in_maps.append(m)
    res = run_bass_kernel_spmd(nc, in_maps, core_ids=list(range(8))).results
    g = lambda c, n: np.asarray(res[c][n], dtype=np.float32)
    y_prompt = np.stack([np.concatenate([g(b, "y")[:1024], g(b + 4, "y")[:1024]], axis=0) for b in range(4)])
    y_sample = np.stack([g(c, "y")[1024:1025] for c in range(8)])
    p_C = np.stack([g(b, "pC") for b in range(4)])[None]
    p_n = np.stack([g(b, "pn") for b in range(4)])[None]
    p_m = np.stack([g(b, "pm")[0] for b in range(4)])[None]
    p_kv = [np.stack([g(b, f"pkv{i}") for b in range(4)]) for i in range(3)]
    s_C = np.stack([g(c, "sC") for c in range(8)])[None]
    s_n = np.stack([g(c, "sn") for c in range(8)])[None]
    s_m = np.stack([g(c, "sm")[0] for c in range(8)])[None]
    s_kv = [np.stack([g(c, "skv")[i][None] for c in range(8)]) for i in range(3)]
    return (y_prompt, y_sample, p_C, p_n, p_m, p_kv[0], p_kv[1], p_kv[2], s_C, s_n, s_m, s_kv[0], s_kv[1], s_kv[2])
```
